# Optimizing a Trainium2 kernel written in Bass

```python
import math
import jax, jax.numpy as jnp
from jax import lax
import numpy as np

D_MODEL = 1024
BATCH = 8
SEQ = 2048
DEPTH = 2

ATTN_HEADS = 8
ATTN_HEAD_DIM = 64
ATTN_WIDTH = ATTN_HEADS * ATTN_HEAD_DIM
DILATED_PATTERNS = ((128, 1), (512, 4), (2048, 16))
BLOCK = 128
NUM_BUCKETS = 32
MAX_DISTANCE = 2048
RET_HEADS = 4
RET_KEY_DIM = 64
RET_VALUE_DIM = 128
RET_WIDTH = RET_HEADS * RET_VALUE_DIM
RET_CHUNK = 128
ROPE_BASE = 10000.0
MIX_WIDTH = ATTN_WIDTH + RET_WIDTH
IN_SPLITS = (ATTN_WIDTH, ATTN_WIDTH, ATTN_WIDTH, RET_HEADS * RET_KEY_DIM,
             RET_HEADS * RET_KEY_DIM, RET_WIDTH, RET_WIDTH)
IN_WIDTH = sum(IN_SPLITS)
D_FF = 2816
N_EXPERTS = 8
TOP_K = 2
D_FF_EXPERT = 3584
N_DENSE = (DEPTH + 1) // 2
N_MOE = DEPTH // 2
EPS = 1e-6
NEG_INF = -1e30

kernel_name = 'hybrid_dilated_retention_moe_block'


def rms_norm(x, gain):
    xf = x.astype(jnp.float32)
    y = xf * lax.rsqrt(jnp.mean(xf * xf, axis=-1, keepdims=True) + EPS)
    return y * gain.astype(jnp.float32)


def t5_bucket(distance):
    max_exact = NUM_BUCKETS // 2
    n = jnp.maximum(distance, 0)
    nf = jnp.maximum(n.astype(jnp.float32), float(max_exact))
    large = max_exact + (jnp.log(nf / max_exact) / math.log(MAX_DISTANCE / max_exact)
                         * (NUM_BUCKETS - max_exact)).astype(jnp.int32)
    large = jnp.minimum(large, NUM_BUCKETS - 1)
    return jnp.where(n < max_exact, n, large)


def dilated_branch(q, k, v, bias_table, window, dilation):
    B, S, H, E = q.shape
    L = S // dilation
    w_sub = window // dilation
    nb = -(-L // BLOCK)
    pad_r = nb * BLOCK - L

    def to_sub(t):
        return t.reshape(B, L, dilation, H, E).transpose(0, 2, 3, 1, 4)

    qb = jnp.pad(to_sub(q), ((0, 0), (0, 0), (0, 0), (0, pad_r), (0, 0)))
    qb = qb.reshape(B, dilation, H, nb, BLOCK, E)

    def kv_windows(t):
        t = jnp.pad(to_sub(t), ((0, 0), (0, 0), (0, 0), (BLOCK, pad_r), (0, 0)))
        t = t.reshape(B, dilation, H, nb + 1, BLOCK, E)
        return jnp.concatenate([t[:, :, :, :-1], t[:, :, :, 1:]], axis=4)

    kw = kv_windows(k)
    vw = kv_windows(v)
    i = jnp.arange(BLOCK)[:, None]
    j = jnp.arange(2 * BLOCK)[None, :]
    rel = i - j + BLOCK
    kpos = jnp.arange(nb)[:, None, None] * BLOCK + j - BLOCK
    allowed = (rel >= 0) & (rel <= w_sub) & (kpos >= 0)
    bias = jnp.transpose(bias_table[t5_bucket(rel * dilation)], (2, 0, 1)).astype(jnp.float32)

    s = jnp.einsum('bdhnqe,bdhnke->bdhnqk', qb, kw) * (E ** -0.5) + bias[None, None, :, None]
    s = jnp.where(allowed[None, None, None], s, NEG_INF)
    m = jnp.max(s, axis=-1, keepdims=True)
    p = jnp.exp(s - m)
    den = jnp.sum(p, axis=-1, keepdims=True)
    o = jnp.einsum('bdhnqk,bdhnke->bdhnqe', p, vw) / den
    lse = (m + jnp.log(den))[..., 0]

    def from_sub(t):
        rest = t.shape[5:]
        t = t.reshape((B, dilation, H, nb * BLOCK) + rest)[:, :, :, :L]
        t = jnp.moveaxis(t, 3, 1)
        return t.reshape((B, S, H) + rest)

    return from_sub(o), from_sub(lse)


def dilated_attention(q, k, v, bias_table):
    outs, lses = [], []
    for window, dilation in DILATED_PATTERNS:
        o, l = dilated_branch(q, k, v, bias_table, window, dilation)
        outs.append(o)
        lses.append(l)
    outs = jnp.stack(outs, axis=0)
    weights = jax.nn.softmax(jnp.stack(lses, axis=0), axis=0)
    return jnp.sum(weights[..., None] * outs, axis=0)


def rotary(t, pos):
    half = t.shape[-1] // 2
    inv = ROPE_BASE ** (-jnp.arange(half, dtype=jnp.float32) / half)
    ang = pos[:, None] * inv[None, :]
    cos = jnp.cos(ang)[None, :, None, :]
    sin = jnp.sin(ang)[None, :, None, :]
    t1, t2 = t[..., :half], t[..., half:]
    return jnp.concatenate([t1 * cos - t2 * sin, t1 * sin + t2 * cos], axis=-1)


def retention_chunkwise(q, k, v):
    B, S, H, Dk = q.shape
    Dv = v.shape[-1]
    nc = S // RET_CHUNK
    log_g = jnp.log(1.0 - 2.0 ** (-5.0 - jnp.arange(H, dtype=jnp.float32)))
    idx = jnp.arange(RET_CHUNK, dtype=jnp.float32)
    diff = idx[:, None] - idx[None, :]
    decay_mask = jnp.where(diff >= 0, jnp.exp(jnp.maximum(diff, 0.0)[None] * log_g[:, None, None]), 0.0)
    q_decay = jnp.exp((idx + 1.0)[None, :] * log_g[:, None])[..., None]
    k_decay = jnp.exp((RET_CHUNK - 1.0 - idx)[None, :] * log_g[:, None])[..., None]
    chunk_decay = jnp.exp(RET_CHUNK * log_g)[:, None, None]

    def chunks(t):
        return t.reshape(B, nc, RET_CHUNK, H, t.shape[-1]).transpose(1, 0, 3, 2, 4)

    def step(state, inp):
        qi, ki, vi = inp
        inner = jnp.einsum('bhqd,bhkd->bhqk', qi, ki) * decay_mask
        inner = jnp.einsum('bhqk,bhkv->bhqv', inner, vi)
        cross = jnp.einsum('bhqd,bhdv->bhqv', qi * q_decay, state)
        new_state = state * chunk_decay + jnp.einsum('bhkd,bhkv->bhdv', ki * k_decay, vi)
        return new_state, inner + cross

    state0 = jnp.zeros((B, H, Dk, Dv), jnp.float32)
    _, ys = lax.scan(step, state0, (chunks(q), chunks(k), chunks(v)))
    return ys.transpose(1, 0, 3, 2, 4).reshape(B, S, H, Dv)


def token_mixer(h, w_in, q_gain, k_gain, ret_gain, w_out, bias_table):
    B, S, _ = h.shape
    proj = jnp.einsum('bsd,de->bse', h, w_in).astype(jnp.float32)
    q, k, v, rq, rk, rv, rg = jnp.split(proj, np.cumsum(IN_SPLITS)[:-1].tolist(), axis=-1)
    q = rms_norm(q.reshape(B, S, ATTN_HEADS, ATTN_HEAD_DIM), q_gain)
    k = rms_norm(k.reshape(B, S, ATTN_HEADS, ATTN_HEAD_DIM), k_gain)
    v = v.reshape(B, S, ATTN_HEADS, ATTN_HEAD_DIM)
    attn = dilated_attention(q, k, v, bias_table).reshape(B, S, ATTN_WIDTH)
    pos = jnp.arange(S, dtype=jnp.float32)
    rq = rotary(rq.reshape(B, S, RET_HEADS, RET_KEY_DIM), pos) * (RET_KEY_DIM ** -0.5)
    rk = rotary(rk.reshape(B, S, RET_HEADS, RET_KEY_DIM), pos)
    rv = rv.reshape(B, S, RET_HEADS, RET_VALUE_DIM)
    ret = retention_chunkwise(rq, rk, rv)
    mu = jnp.mean(ret, axis=-1, keepdims=True)
    var = jnp.mean(jnp.square(ret - mu), axis=-1, keepdims=True)
    ret = ((ret - mu) * lax.rsqrt(var + EPS)).reshape(B, S, RET_WIDTH) * ret_gain.astype(jnp.float32)
    ret = jax.nn.silu(rg) * ret
    mixed = jnp.concatenate([attn, ret], axis=-1).astype(h.dtype)
    return jnp.einsum('bsm,md->bsd', mixed, w_out)


def swiglu(h, w1, w3, w2):
    a = jnp.einsum('bsd,df->bsf', h, w1)
    b = jnp.einsum('bsd,df->bsf', h, w3)
    return jnp.einsum('bsf,fd->bsd', jax.nn.silu(a) * b, w2)


def moe_swiglu(h, router, w1, w3, w2):
    logits = jnp.einsum('bsd,de->bse', h, router).astype(jnp.float32)
    top_vals, top_idx = lax.top_k(logits, TOP_K)
    top_w = jax.nn.softmax(top_vals, axis=-1)
    gates = jnp.sum(jax.nn.one_hot(top_idx, N_EXPERTS, dtype=jnp.float32) * top_w[..., None], axis=-2)
    out = jnp.zeros(h.shape, jnp.float32)
    for e in range(N_EXPERTS):
        out = out + gates[..., e:e + 1] * swiglu(h, w1[e], w3[e], w2[e]).astype(jnp.float32)
    return out


def setup_inputs(seed: int = 0) -> dict:
    key = jax.random.key(seed)
    ks = jax.random.split(key, 20)
    f32 = jnp.float32
    nrm = lambda k, shape, scale: jax.random.normal(k, shape, f32) * scale
    return {
        'x': nrm(ks[0], (BATCH, SEQ, D_MODEL), 1.0),
        'c': nrm(ks[1], (BATCH, D_MODEL), 1.0),
        'rel_bias_table': nrm(ks[2], (NUM_BUCKETS, ATTN_HEADS), 0.3),
        'norm_mix': 1.0 + nrm(ks[3], (DEPTH, D_MODEL), 0.02),
        'norm_ffn': 1.0 + nrm(ks[4], (DEPTH, D_MODEL), 0.02),
        'w_mod': nrm(ks[5], (DEPTH, D_MODEL, 6 * D_MODEL), 0.3 * D_MODEL ** -0.5),
        'b_mod': nrm(ks[6], (DEPTH, 6 * D_MODEL), 0.02),
        'w_in': nrm(ks[7], (DEPTH, D_MODEL, IN_WIDTH), D_MODEL ** -0.5),
        'q_gain': 1.0 + nrm(ks[8], (DEPTH, ATTN_HEAD_DIM), 0.02),
        'k_gain': 1.0 + nrm(ks[9], (DEPTH, ATTN_HEAD_DIM), 0.02),
        'ret_gain': 1.0 + nrm(ks[10], (DEPTH, RET_WIDTH), 0.02),
        'w_out': nrm(ks[11], (DEPTH, MIX_WIDTH, D_MODEL), MIX_WIDTH ** -0.5),
        'ffn_w1': nrm(ks[12], (N_DENSE, D_MODEL, D_FF), D_MODEL ** -0.5),
        'ffn_w3': nrm(ks[13], (N_DENSE, D_MODEL, D_FF), D_MODEL ** -0.5),
        'ffn_w2': nrm(ks[14], (N_DENSE, D_FF, D_MODEL), D_FF ** -0.5),
        'moe_router': nrm(ks[15], (N_MOE, D_MODEL, N_EXPERTS), D_MODEL ** -0.5),
        'moe_w1': nrm(ks[16], (N_MOE, N_EXPERTS, D_MODEL, D_FF_EXPERT), D_MODEL ** -0.5),
        'moe_w3': nrm(ks[17], (N_MOE, N_EXPERTS, D_MODEL, D_FF_EXPERT), D_MODEL ** -0.5),
        'moe_w2': nrm(ks[18], (N_MOE, N_EXPERTS, D_FF_EXPERT, D_MODEL), D_FF_EXPERT ** -0.5),
    }


def reference(x, c, rel_bias_table, norm_mix, norm_ffn, w_mod, b_mod, w_in, q_gain, k_gain,
              ret_gain, w_out, ffn_w1, ffn_w3, ffn_w2, moe_router, moe_w1, moe_w3, moe_w2):
    dt = x.dtype
    c_act = jax.nn.silu(c.astype(jnp.float32))
    for layer in range(DEPTH):
        mod = jnp.einsum('bd,de->be', c_act, w_mod[layer].astype(jnp.float32)) + b_mod[layer].astype(jnp.float32)
        shift_m, scale_m, gate_m, shift_f, scale_f, gate_f = jnp.split(mod[:, None, :], 6, axis=-1)
        h = (rms_norm(x, norm_mix[layer]) * (1.0 + scale_m) + shift_m).astype(dt)
        mix = token_mixer(h, w_in[layer], q_gain[layer], k_gain[layer], ret_gain[layer],
                          w_out[layer], rel_bias_table)
        x = x + (gate_m * mix.astype(jnp.float32)).astype(dt)
        h = (rms_norm(x, norm_ffn[layer]) * (1.0 + scale_f) + shift_f).astype(dt)
        if layer % 2 == 0:
            i = layer // 2
            f = swiglu(h, ffn_w1[i], ffn_w3[i], ffn_w2[i])
        else:
            i = layer // 2
            f = moe_swiglu(h, moe_router[i], moe_w1[i], moe_w3[i], moe_w2[i])
        x = x + (gate_f * f.astype(jnp.float32)).astype(dt)
    return x
```

```python
import math
import numpy as np
import concourse.bass as bass
import concourse.mybir as mybir
from concourse.bass_utils import run_bass_kernel_spmd

F32 = mybir.dt.float32
BF16 = mybir.dt.bfloat16
AF = mybir.ActivationFunctionType
ALU = mybir.AluOpType
AX = mybir.AxisListType

D = 1024
S = 2048
NT = 16
EPS = 1e-6
DFF = 2816
NEXP = 8
DFE = 3584
PATTERNS = ((128, 1), (512, 4), (2048, 16))


class Tick:
    __slots__ = ("sem", "val", "key")

    def __init__(self, sem, val, key):
        self.sem, self.val, self.key = sem, val, key


class Buf:
    def __init__(self, name, excl=False):
        self.name = name
        self.w = {}
        self.r = {}
        self.excl = excl
        self.dsem = None
        self.dcount = 0


class Eng:
    def __init__(self, name, h, sem):
        self.name, self.h, self.sem = name, h, sem
        self.count = 0
        self.seen = {}


class K:
    def __init__(self, nc):
        self.nc = nc
        self.E = {}
        for name, h in (("pe", nc.tensor), ("act", nc.scalar), ("dve", nc.vector),
                        ("pool", nc.gpsimd), ("sp", nc.sync)):
            self.E[name] = Eng(name, h, nc.alloc_semaphore("sem_" + name))
        self.dbufs = []
        self.nbuf = 0

    def buf(self, name, excl=False):
        self.nbuf += 1
        return Buf("%s_%d" % (name, self.nbuf), excl)

    def _deps(self, reads, writes):
        deps = {}

        def add(t):
            o = deps.get(t.key)
            if o is None or o.val < t.val:
                deps[t.key] = t
        for b in reads:
            for t in b.w.values():
                add(t)
            if b.excl:
                for t in b.r.values():
                    add(t)
        for b in writes:
            for t in b.w.values():
                add(t)
            for t in b.r.values():
                add(t)
        return deps

    def _wait(self, E, deps):
        for key, t in deps.items():
            if key == "pe" and E.name == "pe":
                continue
            if E.seen.get(key, 0) >= t.val:
                continue
            E.h.wait_ge(t.sem, t.val)
            E.seen[key] = t.val

    def _mark(self, t, reads, writes):
        for b in reads:
            o = b.r.get(t.key)
            if o is None or o.val < t.val:
                b.r[t.key] = t
        for b in writes:
            b.w = {t.key: t}
            b.r = {}

    def op(self, en, fn, reads=(), writes=()):
        E = self.E[en]
        self._wait(E, self._deps(reads, writes))
        ins = fn(E.h)
        E.count += 1
        ins.then_inc(E.sem, 1)
        t = Tick(E.sem, E.count, E.name)
        self._mark(t, reads, writes)
        return t

    def dma(self, q, out, in_, reads=(), writes=()):
        E = self.E[q]
        self._wait(E, self._deps(reads, writes))
        dst = writes[0]
        if dst.dsem is None:
            dst.dsem = self.nc.alloc_semaphore("d_" + dst.name)
            self.dbufs.append(dst)
        dst.dcount += 1
        if hasattr(in_, "_ap"):
            in_ = in_._ap()
        E.h.dma_start(out=out, in_=in_).then_inc(dst.dsem, 16)
        t = Tick(dst.dsem, 16 * dst.dcount, "dma:" + dst.name)
        for b in reads:
            b.r[t.key] = t
        for b in writes:
            b.w = {k: v for k, v in b.w.items() if k == t.key}
            b.w[t.key] = t
            b.r = {}
        return t

    def barrier(self):
        ticks = {}
        for E in self.E.values():
            if E.count > 0:
                ticks[E.name] = Tick(E.sem, E.count, E.name)
        for b in self.dbufs:
            ticks["dma:" + b.name] = Tick(b.dsem, 16 * b.dcount, "dma:" + b.name)
        for E in self.E.values():
            for key, t in ticks.items():
                if E.seen.get(key, 0) >= t.val:
                    continue
                E.h.wait_ge(t.sem, t.val)
                E.seen[key] = t.val


def bcast_last(ap, n):
    return bass.AP(ap.tensor, ap.offset, [list(d) for d in ap.ap] + [[0, n]])


def bcast_mid(ap, n):
    d = [list(x) for x in ap.ap]
    return bass.AP(ap.tensor, ap.offset, [d[0], [0, n]] + d[1:])


def _t5_bucket(n):
    n = np.maximum(n, 0)
    nf = np.maximum(n.astype(np.float32), np.float32(16.0))
    large = 16 + (np.log(nf / np.float32(16.0)) / np.float32(math.log(2048 / 16)) * np.float32(16)).astype(np.int32)
    large = np.minimum(large, 31)
    return np.where(n < 16, n, large)


def host_consts():
    c = {}
    c["c_ident"] = np.eye(128, dtype=np.float32)
    blk = np.zeros((128, 128), np.float32)
    blk[:64, :64] = 1
    blk[64:, 64:] = 1
    c["c_blk"] = blk
    half = 32
    inv = (np.float32(10000.0) ** (-np.arange(half, dtype=np.float32) / np.float32(half))).astype(np.float32)
    pos = np.arange(S, dtype=np.float32)
    ang = (pos[:, None] * inv[None, :]).astype(np.float32)
    cs = np.concatenate([np.cos(ang), np.sin(ang)], axis=1).astype(np.float32)
    c["c_cs"] = np.ascontiguousarray(cs.reshape(NT, 128, 64).transpose(1, 0, 2))
    lg = np.log(1.0 - 2.0 ** (-5.0 - np.arange(4, dtype=np.float64)))
    idx = np.arange(128, dtype=np.float64)
    qdec = np.exp((idx[:, None] + 1.0) * lg[None, :]) * 0.125
    kdec = np.exp((127.0 - idx[:, None]) * lg[None, :])
    c["c_dec"] = np.concatenate([qdec, kdec], axis=1).astype(np.float32)
    kk = idx[:, None, None]
    qq = idx[None, None, :]
    m2 = np.exp(-(kk + 1.0) * lg[None, :, None]) * (qq >= kk)
    c["c_mask2"] = m2.astype(np.float32)
    oh = np.zeros((33, 3, 384), np.float32)
    for b, (_, d) in enumerate(PATTERNS):
        m = np.arange(384)
        rel = m - 127
        ok = (rel >= 0) & (rel <= 128)
        bk = _t5_bucket(rel * d)
        for mm in range(384):
            if ok[mm]:
                oh[bk[mm], b, mm] = 1.0
            else:
                oh[32, b, mm] = 1.0
    c["c_oh"] = oh
    return c


CHUNK_DECAY = [float((1.0 - 2.0 ** (-5.0 - h)) ** 128) for h in range(4)]


def build(sublayers=((0, "mix"), (0, "ffn"), (1, "mix"), (1, "ffn")), stop=None):
    nc = bass.Bass("TRN2", target_bir_lowering=False)
    k = K(nc)

    uid = [0]

    def sb(name, shape, dt):
        uid[0] += 1
        return nc.sbuf_tensor("s%d_%s" % (uid[0], name), shape, dt)

    def sba(name, shape, dt):
        uid[0] += 1
        return nc.alloc_sbuf_tensor("s%d_%s" % (uid[0], name), shape, dt)

    shapes = {
        "x": (S, D), "cT": (128, 8), "rel_bias_table": (32, 8), "norm_mix": (2, D), "norm_ffn": (2, D),
        "w_mod": (2, D, 6 * D), "b_mod": (2, 6 * D), "w_in": (2, D, 3072), "q_gain2": (128, 2), "k_gain2": (128, 2),
        "ret_gain": (2, 512), "w_out": (2, D, D), "ffn_w1": (D, DFF), "ffn_w3": (D, DFF), "ffn_w2": (DFF, D),
        "moe_router": (D, NEXP), "moe_w1": (NEXP, D, DFE), "moe_w3": (NEXP, D, DFE), "moe_w2": (NEXP, DFE, D),
        "c_ident": (128, 128), "c_blk": (128, 128), "c_cs": (128, NT, 64), "c_dec": (128, 8),
        "c_mask2": (128, 4, 128), "c_oh": (33, 3, 384),
    }
    used = {}

    class Lazy:
        def __init__(self, name):
            self.name = name

        def _ap(self):
            if self.name not in used:
                used[self.name] = nc.dram_tensor(self.name, list(shapes[self.name]), F32, kind="ExternalInput").ap()
            return used[self.name]

        def __getitem__(self, key):
            return self._ap()[key]

        def rearrange(self, *a, **kw):
            return self._ap().rearrange(*a, **kw)

        @property
        def tensor(self):
            return self._ap().tensor

        def ap(self):
            return self._ap()

    x_d, cT_d, table_d, nmix_d, nffn_d = Lazy("x"), Lazy("cT"), Lazy("rel_bias_table"), Lazy("norm_mix"), Lazy("norm_ffn")
    wmod_d, bmod_d, win_d, qg_d, kg_d = Lazy("w_mod"), Lazy("b_mod"), Lazy("w_in"), Lazy("q_gain2"), Lazy("k_gain2")
    rgain_d, wout_d, w1_d, w3_d, w2_d = Lazy("ret_gain"), Lazy("w_out"), Lazy("ffn_w1"), Lazy("ffn_w3"), Lazy("ffn_w2")
    router_d, mw1_d, mw3_d, mw2_d = Lazy("moe_router"), Lazy("moe_w1"), Lazy("moe_w3"), Lazy("moe_w2")
    cid_d, cblk_d, ccs_d, cdec_d, cm2_d, coh_d = (Lazy("c_ident"), Lazy("c_blk"), Lazy("c_cs"), Lazy("c_dec"),
                                                  Lazy("c_mask2"), Lazy("c_oh"))
    out_d = nc.dram_tensor("out", [S, D], F32, kind="ExternalOutput").ap()
    ebscr = nc.dram_tensor("ebscr", [24, 128, 384], F32, kind="Internal")
    ebscr_buf = k.buf("ebscr")
    out_buf = k.buf("outd")

    x = sba("x", [128, NT, D], F32)
    hT = sba("hT", [128, 8, S], BF16)
    ident = sba("ident", [128, 128], BF16)
    blk = sba("blk", [128, 128], BF16)
    ones = sba("ones", [128, 128], BF16)
    onesf = sba("onesf", [1, 128], F32)
    cs = sba("cs", [128, NT, 64], F32)
    dec = sba("dec", [128, 8], F32)
    mask2 = sba("mask2", [128, 4, 128], F32)
    cact = sba("cact", [128, 8], F32)
    cact_rep = sba("cact_rep", [128, 8, 128], F32)
    gains = sba("gains", [128, 4], F32)
    gate = sba("gate", [128, D], F32)
    ss = sba("ss", [128, NT], F32)
    rs = sba("rs", [128, NT], F32)
    ps = nc.alloc_psum_tensor("ps", [128, 8, 512], F32)
    PB = [k.buf("psb%d" % i, excl=True) for i in range(8)]

    B = {n: k.buf(n) for n in ("x", "hT", "const", "cact", "gains", "gate", "ss", "rs")}
    XT = [k.buf("xt%d" % t) for t in range(NT)]

    def psbf(i):
        return ps[:, i, :].bitcast(BF16)

    for t4 in range(4):
        k.dma("sp", x[:, 4 * t4:4 * t4 + 4, :],
              x_d[512 * t4:512 * (t4 + 1), :].rearrange("(t p) d -> p t d", p=128),
              writes=[B["x"]])
    Bcp = k.buf("constp")
    k.dma("pool", ident[:], cid_d, writes=[Bcp])
    k.dma("pool", blk[:], cblk_d, writes=[Bcp])
    k.dma("sp", cs[:], ccs_d, writes=[B["const"]])
    k.dma("sp", dec[:], cdec_d, writes=[B["const"]])
    k.dma("sp", mask2[:], cm2_d, writes=[B["const"]])
    k.dma("sp", cact[:], cT_d, writes=[B["cact"]])
    k.dma("sp", gains[:, 0:2], qg_d, writes=[B["gains"]])
    k.dma("sp", gains[:, 2:4], kg_d, writes=[B["gains"]])
    k.op("dve", lambda e: e.memset(ones[:], 1.0), reads=[Bcp], writes=[B["const"]])
    k.op("dve", lambda e: e.memset(onesf[:], 1.0), writes=[B["const"]])
    k.op("dve", lambda e: e.tensor_scalar(out=gains[:], in0=gains[:], scalar1=8.0, scalar2=None, op0=ALU.mult),
         reads=[B["gains"]], writes=[B["gains"]])
    k.op("act", lambda e: e.activation(out=cact[:], in_=cact[:], func=AF.Silu), reads=[B["cact"]], writes=[B["cact"]])
    for kc in range(8):
        k.op("dve", lambda e, kc=kc: e.tensor_copy(out=cact_rep[:, kc, :], in_=cact[:, kc:kc + 1].to_broadcast([128, 128])),
             reads=[B["cact"]], writes=[B["const"]])
    for XB in XT:
        XB.w = dict(B["x"].w)

    if any(sl == "mix" for _, sl in sublayers):
        with (sb("tab", [33, 8], F32) as tab, sb("tabb", [33, 8, 128], F32) as tabb,
              sb("oh", [33, 3, 384], F32) as oh, sb("ebst", [128, 2, 384], F32) as ebst):
            Bt = k.buf("tab")
            Bst = [k.buf("ebst0"), k.buf("ebst1")]
            k.op("dve", lambda e: e.memset(tab[:], -30000.0), writes=[Bt])
            k.dma("sp", tab[0:32, :], table_d, writes=[Bt])
            k.dma("sp", oh[:], coh_d, writes=[Bt])
            k.op("dve", lambda e: e.tensor_copy(out=tabb[:], in_=bcast_last(tab[:], 128)), reads=[Bt], writes=[Bt])
            i = 0
            for b in range(3):
                for h in range(8):
                    bank = i % 2
                    k.op("pe", lambda e: e.matmul(ps[:, bank, 0:384], lhsT=tabb[:, h, :], rhs=oh[:, b, :], start=True, stop=True),
                         reads=[Bt], writes=[PB[bank]])
                    k.op("act", lambda e: e.activation(out=ebst[:, bank, :], in_=ps[:, bank, 0:384], func=AF.Exp),
                         reads=[PB[bank]], writes=[Bst[bank]])
                    k.dma("sp", bass.AP(ebscr, (b * 8 + h) * 128 * 384, [[384, 128], [1, 384]]),
                          ebst[:, bank, :], reads=[Bst[bank]], writes=[ebscr_buf])
                    i += 1
            k.barrier()

    def dump(name, t, dt, bufs):
        dd = nc.dram_tensor("dbg_" + name, list(t.shape), dt, kind="ExternalOutput").ap()
        k.dma("sp", dd, t[:], reads=bufs, writes=[out_buf])

    def mod_and_norm(l, which, want_f32=None):
        base = 0 if which == "mix" else 3
        ND = 2
        norm_d = nmix_d if which == "mix" else nffn_d
        with (sb("A", [128, D], F32) as A, sb("shift", [128, D], F32) as shift,
              sb("normB", [128, D], F32) as normB, sb("wm", [128, 2, 8, 512], F32) as wm,
              sb("bm", [1, 3 * D], F32) as bm, sb("junk", [128, D], BF16) as junk,
              sb("tmp", [128, ND, D], F32) as tmp, sb("hbf", [128, ND, D], BF16) as hbf,
              sb("sq", [128, NT], F32) as sqv):
            BA, Bs, Bn, Bbm, Bj = k.buf("A"), k.buf("shift"), k.buf("normB"), k.buf("bm"), k.buf("junk")
            Bwm = [k.buf("wm0"), k.buf("wm1")]
            Btmp = [k.buf("tmp%d" % q) for q in range(ND)]
            Bhbf = [k.buf("hbf%d" % q) for q in range(ND)]
            k.dma("sp", normB[:], bass.AP(norm_d.tensor, l * D, [[0, 128], [1, D]]), writes=[Bn])
            k.dma("sp", bm[:], bmod_d[l:l + 1, base * D:(base + 3) * D], writes=[Bbm])
            k.op("dve", lambda e: e.tensor_scalar(out=normB[:], in0=normB[:], scalar1=32.0, scalar2=None, op0=ALU.mult),
                 reads=[Bn], writes=[Bn])
            for t in range(NT):
                k.op("act", lambda e: e.activation(out=junk[:], in_=x[:, t, :], func=AF.Square, accum_out=ss[:, t:t + 1]),
                     reads=[XT[t]], writes=[Bj, B["ss"]])
            k.op("act", lambda e: e.activation(out=sqv[:], in_=ss[:], func=AF.Sqrt, bias=float(D * EPS)), reads=[B["ss"]], writes=[B["rs"]])
            k.op("dve", lambda e: e.reciprocal(out=rs[:], in_=sqv[:]), reads=[B["rs"]], writes=[B["rs"]])
            for nb in range(6):
                slot = nb % 2
                col0 = base * D + nb * 512
                k.dma("sp", wm[:, slot], wmod_d[l, :, col0:col0 + 512].rearrange("(c p) n -> p c n", p=128), writes=[Bwm[slot]])
                bank = nb % 2

                def mm(e):
                    for kc in range(8):
                        e.matmul(ps[:, bank, :], lhsT=cact_rep[:, kc, :], rhs=wm[:, slot, kc, :], start=(kc == 0), stop=False)
                    return e.matmul(ps[:, bank, :], lhsT=onesf[0:1, :], rhs=bm[0:1, nb * 512:(nb + 1) * 512], start=False, stop=True)
                k.op("pe", mm, reads=[Bwm[slot], Bbm, B["const"]], writes=[PB[bank]])
                half = slice((nb % 2) * 512, (nb % 2) * 512 + 512)
                kind = nb // 2
                if kind == 0:
                    k.op("act", lambda e: e.copy(out=shift[:, half], in_=ps[:, bank, :]), reads=[PB[bank]], writes=[Bs])
                elif kind == 1:
                    k.op("dve", lambda e: e.scalar_tensor_tensor(out=A[:, half], in0=ps[:, bank, :], scalar=1.0, in1=normB[:, half],
                                                                 op0=ALU.add, op1=ALU.mult), reads=[PB[bank], Bn], writes=[BA])
                else:
                    k.op("act", lambda e: e.copy(out=gate[:, half], in_=ps[:, bank, :]), reads=[PB[bank]], writes=[B["gate"]])
            for t in range(NT):
                s2 = t % ND
                k.op("dve", lambda e: e.scalar_tensor_tensor(out=tmp[:, s2, :], in0=x[:, t, :], scalar=rs[:, t:t + 1], in1=A[:],
                                                             op0=ALU.mult, op1=ALU.mult), reads=[XT[t], B["rs"], BA], writes=[Btmp[s2]])
                if want_f32 is not None:
                    k.op("dve", lambda e: e.tensor_tensor(out=tmp[:, s2, :], in0=tmp[:, s2, :], in1=shift[:], op=ALU.add),
                         reads=[Btmp[s2], Bs], writes=[Btmp[s2]])
                    k.op("act", lambda e: e.copy(out=hbf[:, s2, :], in_=tmp[:, s2, :]), reads=[Btmp[s2]], writes=[Bhbf[s2]])
                else:
                    k.op("dve", lambda e: e.tensor_tensor(out=hbf[:, s2, :], in0=tmp[:, s2, :], in1=shift[:], op=ALU.add),
                         reads=[Btmp[s2], Bs], writes=[Bhbf[s2]])
                bank = 2 + (t % ND)

                def tr(e):
                    for kc in range(8):
                        ins = e.transpose(out=psbf(bank)[:, kc * 128:(kc + 1) * 128], in_=hbf[:, s2, kc * 128:(kc + 1) * 128], identity=ident[:])
                    return ins
                k.op("pe", tr, reads=[Bhbf[s2], B["const"]], writes=[PB[bank]])
                k.op("act", lambda e: e.copy(out=hT[:, :, t * 128:(t + 1) * 128], in_=psbf(bank).rearrange("p (c n) -> p c n", c=8)),
                     reads=[PB[bank]], writes=[B["hT"]])
                if want_f32 is not None:
                    want_f32(t, tmp, s2, Btmp[s2], hbf, Bhbf[s2])
            k.barrier()

    def residual_update(t, psrc, banks, tmpt, Btmpt, scalar=None):
        src = ps[:, banks[0]:banks[0] + 2, :].rearrange("p a n -> p (a n)")
        if scalar is None:
            k.op("dve", lambda e: e.tensor_tensor(out=tmpt, in0=src, in1=gate[:], op=ALU.mult),
                 reads=[PB[banks[0]], PB[banks[1]], B["gate"]], writes=[Btmpt])
        else:
            k.op("dve", lambda e: e.scalar_tensor_tensor(out=tmpt, in0=src, scalar=scalar[0], in1=gate[:], op0=ALU.mult, op1=ALU.mult),
                 reads=[PB[banks[0]], PB[banks[1]], B["gate"], scalar[1]], writes=[Btmpt])
        k.op("pool", lambda e: e.tensor_tensor(out=x[:, t, :], in0=x[:, t, :], in1=tmpt, op=ALU.add),
             reads=[Btmpt, XT[t]], writes=[XT[t]])

    def mixer(l):
        with sb("attnT", [128, 4, S], BF16) as attnT:
            BattnT = k.buf("attnT")
            if stop == "modnorm":
                dump("hT", hT, BF16, [B["hT"]])
                dump("gate", gate, F32, [B["gate"]])
                return
            attention(l, attnT, BattnT)
            if stop == "attn_proj" or (stop and stop.startswith("attn_units:")):
                return
            if stop == "attn":
                dump("attnT", attnT, BF16, [BattnT])
                return
            with sb("retT", [128, 4, S], BF16) as retT:
                BretT = k.buf("retT")
                retention(l, retT, BretT)
                if stop == "ret":
                    dump("retT", retT, BF16, [BretT])
                    return
                with (sb("wo", [128, 8, D], BF16) as wo, sb("tmpo", [128, 4, D], F32) as tmpo):
                    Bwo = k.buf("wo")
                    Bt2 = [k.buf("tmpo%d" % q) for q in range(4)]
                    for hf in range(2):
                        k.dma("pool", wo[:, :, hf * 512:(hf + 1) * 512],
                              wout_d[l, :, hf * 512:(hf + 1) * 512].rearrange("(c p) n -> p c n", p=128), writes=[Bwo])
                    for t in range(NT):
                        b0 = 2 * (t % 4)

                        def mm(e):
                            for hf in range(2):
                                for kc in range(8):
                                    src = attnT if kc < 4 else retT
                                    ins = e.matmul(ps[:, b0 + hf, :], lhsT=src[:, kc % 4, t * 128:(t + 1) * 128], rhs=wo[:, kc, hf * 512:(hf + 1) * 512],
                                                   start=(kc == 0), stop=(kc == 7))
                            return ins
                        k.op("pe", mm, reads=[BattnT, BretT, Bwo], writes=[PB[b0], PB[b0 + 1]])
                        residual_update(t, None, (b0, b0 + 1), tmpo[:, t % 4, :], Bt2[t % 4])
                    k.barrier()

    def attention(l, mixT, BmixT):
        with (sb("wqkv", [128, 2, 8, 3, 128], BF16) as wqkv, sb("qT", [128, S], BF16) as qT,
              sb("kT", [128, S], BF16) as kT, sb("V", [128, 3, 16, 128], BF16) as V,
              sb("EB", [128, 3, 2, 256], BF16) as EB, sb("acc", [128, 2, S], F32) as acc,
              sb("esb", [128, 3, 2, 256], BF16) as esb, sb("psb", [128, 3, 2, 256], BF16) as psb,
              sb("sqb", [128, 2, 512], BF16) as sqb, sb("rsb", [128, 2, 512], F32) as rsb,
              sb("vT", [128, S], BF16) as vT):
            Bw = [k.buf("wqkv0"), k.buf("wqkv1")]
            BqT, BkT, BV, BEB, Bacc = k.buf("qT"), k.buf("kT"), k.buf("V"), k.buf("EB"), k.buf("acc")
            Besb = [k.buf("esb%d" % q) for q in range(3)]
            Bpsb = [k.buf("psb%d" % q) for q in range(3)]
            Bsq = [k.buf("sqb0"), k.buf("sqb1")]
            Brs = [k.buf("rsb0"), k.buf("rsb1")]
            BvT = k.buf("vT")

            def load_w(c):
                s = c % 2
                for j in range(3):
                    col = j * 512 + c * 128
                    k.dma("pool", wqkv[:, s, :, j, :], win_d[l, :, col:col + 128].rearrange("(c p) n -> p c n", p=128), writes=[Bw[s]])
            load_w(0)
            for c in range(4):
                s = c % 2
                if c + 1 < 4:
                    load_w(c + 1)
                for b in range(3):
                    for hh in range(2):
                        i = b * 8 + c * 2 + hh
                        k.dma("pool", EB[:, b, hh, :], bass.AP(ebscr, i * 128 * 384 + 127, [[383, 128], [1, 256]]),
                              reads=[ebscr_buf], writes=[BEB])
                qk_steps, vp_steps, vt_steps = [], [], []
                ci = 0
                for j, (dst, Bdst) in enumerate(((qT, BqT), (kT, BkT))):
                    gcol = gains[:, 2 * j + l:2 * j + l + 1]
                    for tb in range(4):
                        z = ci % 2
                        ci += 1

                        def qk(j=j, dst=dst, Bdst=Bdst, gcol=gcol, tb=tb, z=z):
                            ba, bb = 2 * z, 2 * z + 1

                            def mm(e):
                                for kc in range(8):
                                    ins = e.matmul(ps[:, ba, :], lhsT=wqkv[:, s, kc, j, :], rhs=hT[:, kc, tb * 512:(tb + 1) * 512],
                                                   start=(kc == 0), stop=(kc == 7))
                                return ins
                            k.op("pe", mm, reads=[Bw[s], B["hT"]], writes=[PB[ba]])
                            k.op("act", lambda e: e.activation(out=sqb[:, z, :], in_=ps[:, ba, :], func=AF.Square), reads=[PB[ba]], writes=[Bsq[z]])
                            k.op("pe", lambda e: e.matmul(ps[:, bb, :], lhsT=blk[:], rhs=sqb[:, z, :], start=True, stop=True),
                                 reads=[Bsq[z], B["const"]], writes=[PB[bb]])
                            k.op("act", lambda e: e.activation(out=rsb[:, z, :], in_=ps[:, bb, :], func=AF.Sqrt, bias=float(64 * EPS)),
                                 reads=[PB[bb]], writes=[Brs[z]])
                            k.op("dve", lambda e: e.reciprocal(out=rsb[:, z, :], in_=rsb[:, z, :]), reads=[Brs[z]], writes=[Brs[z]])
                            k.op("dve", lambda e: e.scalar_tensor_tensor(out=dst[:, tb * 512:(tb + 1) * 512], in0=ps[:, ba, :], scalar=gcol,
                                                                         in1=rsb[:, z, :], op0=ALU.mult, op1=ALU.mult),
                                 reads=[PB[ba], Brs[z], B["gains"]], writes=[Bdst])
                        qk_steps.append(qk)
                for tb in range(4):
                    def vp(tb=tb):
                        bk = 4 + (tb % 2)

                        def mm(e):
                            for kc in range(8):
                                ins = e.matmul(ps[:, bk, :], lhsT=wqkv[:, s, kc, 2, :], rhs=hT[:, kc, tb * 512:(tb + 1) * 512],
                                               start=(kc == 0), stop=(kc == 7))
                            return ins
                        k.op("pe", mm, reads=[Bw[s], B["hT"]], writes=[PB[bk]])
                        k.op("act", lambda e: e.copy(out=vT[:, tb * 512:(tb + 1) * 512], in_=ps[:, bk, :]), reads=[PB[bk]], writes=[BvT])
                    vp_steps.append(vp)
                gi = 0
                for b, (_, d) in enumerate(PATTERNS):
                    L = S // d
                    nb = max(L // 128, 1)
                    for g4 in range(4):
                        def vt(b=b, d=d, nb=nb, g4=g4, bk=6 + (gi % 2)):
                            def tr(e):
                                for q4 in range(4):
                                    bi = g4 * 4 + q4
                                    r, n = bi // nb, bi % nb
                                    t0 = r + d * 128 * n
                                    ins = e.transpose(out=psbf(bk)[:, q4 * 128:(q4 + 1) * 128], in_=vT[:, t0:t0 + 127 * d + 1:d], identity=ident[:])
                                return ins
                            k.op("pe", tr, reads=[BvT, B["const"]], writes=[PB[bk]])
                            k.op("act", lambda e: e.copy(out=V[:, b, g4 * 4:(g4 + 1) * 4, :], in_=psbf(bk)[:, 0:512].rearrange("p (a n) -> p a n", a=4)),
                                 reads=[PB[bk]], writes=[BV])
                        gi += 1
                        vt_steps.append(vt)
                for i4 in range(4):
                    qk_steps[i4]()
                    vp_steps[i4]()
                for i4 in range(4):
                    qk_steps[4 + i4]()
                    for vtt in vt_steps[3 * i4:3 * i4 + 3]:
                        vtt()
                if stop == "attn_proj":
                    dump("qT", qT, BF16, [BqT]); dump("kT", kT, BF16, [BkT]); dump("V", V, BF16, [BV]); dump("EB", EB, BF16, [BEB])
                    return
                units = []
                for b, (_, d) in enumerate(PATTERNS):
                    L = S // d
                    nb = max(L // 128, 1)
                    for r in range(d):
                        for n in range(nb):
                            units.append((b, d, nb, r, n))

                def scores(u, ui):
                    b, d, nb, r, n = u
                    sb = 2 + 2 * (ui % 2)
                    tq = r + d * 128 * n
                    qs = slice(tq, tq + 127 * d + 1, d)
                    psv = ps[:, sb:sb + 2, 0:256]

                    def mm(e):
                        for hh in range(2):
                            pr = slice(hh * 64, hh * 64 + 64)
                            ins = e.matmul(psv[:, hh, 0:128], lhsT=kT[pr, qs], rhs=qT[pr, qs], start=True, stop=True)
                            if n > 0:
                                tp = r + d * 128 * (n - 1)
                                ins = e.matmul(psv[:, hh, 128:256], lhsT=kT[pr, tp:tp + 127 * d + 1:d], rhs=qT[pr, qs], start=True, stop=True)
                        return ins
                    k.op("pe", mm, reads=[BqT, BkT], writes=[PB[sb], PB[sb + 1]])
                    w = 256 if n > 0 else 128
                    k.op("act", lambda e: e.activation(out=esb[:, ui % 3, :, 0:w], in_=psv[:, :, 0:w], func=AF.Exp, scale=0.125),
                         reads=[PB[sb], PB[sb + 1]], writes=[Besb[ui % 3]])
                    k.op("dve", lambda e: e.tensor_tensor(out=psb[:, ui % 3, :, 0:w], in0=esb[:, ui % 3, :, 0:w], in1=EB[:, b, :, 0:w], op=ALU.mult),
                         reads=[Besb[ui % 3], BEB], writes=[Bpsb[ui % 3]])

                def pv(u, ui):
                    b, d, nb, r, n = u
                    ob = 6 + (ui % 2)
                    pov = ps[:, ob, 0:256].rearrange("p (a n) -> p a n", a=2)
                    bi = r * nb + n

                    def mm(e):
                        for hh in range(2):
                            pr = slice(hh * 64, hh * 64 + 64)
                            for a in range(2):
                                lc = V[:, b, bi, pr] if a == 0 else ones[:, 0:64]
                                ins = e.matmul(pov[pr, a, :], lhsT=lc, rhs=psb[:, ui % 3, hh, 0:128], start=True, stop=(n == 0))
                                if n > 0:
                                    lp = V[:, b, bi - 1, pr] if a == 0 else ones[:, 0:64]
                                    ins = e.matmul(pov[pr, a, :], lhsT=lp, rhs=psb[:, ui % 3, hh, 128:256], start=False, stop=True)
                        return ins
                    k.op("pe", mm, reads=[Bpsb[ui % 3], BV, B["const"]], writes=[PB[ob]])
                    tq = r + d * 128 * n
                    dst = acc[:, :, tq:tq + 127 * d + 1:d]
                    if b == 0:
                        k.op("dve", lambda e: e.tensor_copy(out=dst, in_=pov), reads=[PB[ob]], writes=[Bacc])
                    else:
                        k.op("dve", lambda e: e.tensor_tensor(out=dst, in0=dst, in1=pov, op=ALU.add), reads=[PB[ob], Bacc], writes=[Bacc])
                if stop and stop.startswith("attn_units:"):
                    units = units[:int(stop.split(":")[1])]
                    k.op("dve", lambda e: e.memset(acc[:], 0.0), writes=[Bacc])
                nopv = bool(stop and stop.endswith(":nopv"))
                LAG = 2
                for ui, u in enumerate(units):
                    scores(u, ui)
                    if ui >= LAG and not nopv:
                        pv(units[ui - LAG], ui - LAG)
                if units and not nopv:
                    for uj in range(max(len(units) - LAG, 0), len(units)):
                        pv(units[uj], uj)
                if stop and stop.startswith("attn_units:"):
                    dump("acc", acc, F32, [Bacc]); dump("esb", esb, BF16, Besb); dump("psb", psb, BF16, Bpsb)
                    return
                k.op("dve", lambda e: e.reciprocal(out=acc[:, 1, :], in_=acc[:, 1, :]), reads=[Bacc], writes=[Bacc])
                k.op("dve", lambda e: e.tensor_tensor(out=mixT[:, c, :], in0=acc[:, 0, :], in1=acc[:, 1, :], op=ALU.mult),
                     reads=[Bacc], writes=[BmixT])
            k.barrier()

    def retention(l, retT, BretT):
        with (sb("wr", [128, 2, 8, 384], BF16) as wr, sb("rqd", [128, NT, 64], BF16) as rqd,
              sb("rk", [128, NT, 64], BF16) as rk, sb("rkd", [128, NT, 64], BF16) as rkd,
              sb("rqdT", [64, S], BF16) as rqdT, sb("rkT", [64, S], BF16) as rkT,
              sb("rv", [128, NT, 128], BF16) as rv, sb("sgg", [128, NT, 128], BF16) as sgg,
              sb("rett", [128, NT, 128], BF16) as rett, sb("gB", [128, 512], F32) as gB,
              sb("rot", [128, 6, 4, 2, 32], F32) as rot, sb("sgt", [128, 2, 2, 128], F32) as sgt,
              sb("Sf", [64, 2, 128], F32) as Sf, sb("Sbf", [64, NT, 128], BF16) as Sbf,
              sb("inT", [128, 4, 128], BF16) as inT, sb("sqy", [128, 512], F32) as sqy,
              sb("ty", [128, 2, 512], F32) as ty, sb("st", [128, 2, 8, 4], F32) as st):
            Bw = [k.buf("wr0"), k.buf("wr1")]
            Brqd, Brk, Brkd, BrqdT, BrkT, Brv, Bsgg, Brett, BgB, Brot, Bsqy = (k.buf(n) for n in (
                "rqd", "rk", "rkd", "rqdT", "rkT", "rv", "sgg", "rett", "gB", "rot", "sqy"))
            Bsgt = [k.buf("sgt0"), k.buf("sgt1")]
            BSf = [k.buf("Sf0"), k.buf("Sf1")]
            BSbf = k.buf("Sbf")
            BinT = [k.buf("inT%d" % i) for i in range(4)]
            Bty = [k.buf("ty0"), k.buf("ty1")]
            Bst = [k.buf("st0"), k.buf("st1")]
            k.dma("sp", gB[:], bass.AP(rgain_d.tensor, l * 512, [[0, 128], [1, 512]]), writes=[BgB])

            def load_w(h):
                s = h % 2
                for (c0, w, o) in ((1536 + 64 * h, 64, 0), (1792 + 64 * h, 64, 64), (2048 + 128 * h, 128, 128), (2560 + 128 * h, 128, 256)):
                    k.dma("pool", wr[:, s, :, o:o + w], win_d[l, :, c0:c0 + w].rearrange("(c p) n -> p c n", p=128), writes=[Bw[s]])
            load_w(0)
            for h in range(4):
                s = h % 2
                if h + 1 < 4:
                    load_w(h + 1)
                qd = dec[:, h:h + 1]
                kd = dec[:, 4 + h:5 + h]
                for g4 in range(4):
                    ba = g4 % 2
                    tsl = slice(4 * g4, 4 * g4 + 4)

                    def mm(e):
                        for q4 in range(4):
                            t = 4 * g4 + q4
                            for kc in range(8):
                                ins = e.matmul(ps[:, ba, q4 * 128:(q4 + 1) * 128], lhsT=hT[:, kc, t * 128:(t + 1) * 128], rhs=wr[:, s, kc, 0:128],
                                               start=(kc == 0), stop=(kc == 7))
                        return ins
                    k.op("pe", mm, reads=[Bw[s], B["hT"]], writes=[PB[ba]])
                    pq = ps[:, ba, :].rearrange("p (t a f n) -> p t a f n", t=4, a=2, f=2)
                    t1 = pq[:, :, :, 0, :]
                    t2 = pq[:, :, :, 1, :]
                    cview = cs[:, tsl, 0:32]
                    sview = cs[:, tsl, 32:64]
                    dd = [list(x) for x in cview.ap]
                    cosb = bass.AP(cview.tensor, cview.offset, [dd[0], dd[1], [0, 2], dd[2]])
                    dd = [list(x) for x in sview.ap]
                    sinb = bass.AP(sview.tensor, sview.offset, [dd[0], dd[1], [0, 2], dd[2]])
                    for (ri, ta, tb_) in ((0, t1, cosb), (1, t2, sinb), (2, t1, sinb), (3, t2, cosb)):
                        k.op("dve", lambda e: e.tensor_tensor(out=rot[:, ri], in0=ta, in1=tb_, op=ALU.mult),
                             reads=[PB[ba], B["const"]], writes=[Brot])
                    k.op("dve", lambda e: e.tensor_tensor(out=rot[:, 4], in0=rot[:, 0], in1=rot[:, 1], op=ALU.subtract), reads=[Brot], writes=[Brot])
                    k.op("dve", lambda e: e.tensor_tensor(out=rot[:, 5], in0=rot[:, 2], in1=rot[:, 3], op=ALU.add), reads=[Brot], writes=[Brot])
                    srcq = rot[:, 4:6, :, 0, :].rearrange("p f t n -> p t f n")
                    srck = rot[:, 4:6, :, 1, :].rearrange("p f t n -> p t f n")
                    k.op("act", lambda e: e.activation(out=rqd[:, tsl, :].rearrange("p t (f n) -> p t f n", f=2), in_=srcq, func=AF.Identity, scale=qd),
                         reads=[Brot, B["const"]], writes=[Brqd])
                    k.op("act", lambda e: e.copy(out=rk[:, tsl, :].rearrange("p t (f n) -> p t f n", f=2), in_=srck), reads=[Brot], writes=[Brk])
                    k.op("act", lambda e: e.activation(out=rkd[:, tsl, :].rearrange("p t (f n) -> p t f n", f=2), in_=srck, func=AF.Identity, scale=kd),
                         reads=[Brot], writes=[Brkd])
                    for t2i in range(2):
                        z = (2 * g4 + t2i) % 2
                        bb = 2 + z
                        t0 = 4 * g4 + 2 * t2i

                        def mm2(e):
                            for q2 in range(2):
                                t = t0 + q2
                                for kc in range(8):
                                    ins = e.matmul(ps[:, bb, q2 * 256:(q2 + 1) * 256], lhsT=hT[:, kc, t * 128:(t + 1) * 128], rhs=wr[:, s, kc, 128:384],
                                                   start=(kc == 0), stop=(kc == 7))
                            return ins
                        k.op("pe", mm2, reads=[Bw[s], B["hT"]], writes=[PB[bb]])
                        pv2 = ps[:, bb, :].rearrange("p (q c n) -> p q c n", q=2, c=2)
                        k.op("act", lambda e: e.copy(out=rv[:, t0:t0 + 2, :], in_=pv2[:, :, 0, :]), reads=[PB[bb]], writes=[Brv])
                        k.op("act", lambda e: e.activation(out=sgt[:, z], in_=pv2[:, :, 1, :], func=AF.Silu), reads=[PB[bb]], writes=[Bsgt[z]])
                        k.op("pool", lambda e: e.tensor_tensor(out=sgg[:, t0:t0 + 2, :], in0=sgt[:, z], in1=bcast_mid(gB[:, h * 128:(h + 1) * 128], 2), op=ALU.mult),
                             reads=[Bsgt[z], BgB], writes=[Bsgg])
                gi = 0
                for (src, Bsrc, dstT, BdstT) in ((rqd, Brqd, rqdT, BrqdT), (rk, Brk, rkT, BrkT)):
                    for g4 in range(4):
                        bank = 4 + (gi % 2)
                        gi += 1

                        def tr(e):
                            for q4 in range(4):
                                t = g4 * 4 + q4
                                ins = e.transpose(out=psbf(bank)[0:64, q4 * 128:(q4 + 1) * 128], in_=src[:, t, :], identity=ident[:])
                            return ins
                        k.op("pe", tr, reads=[Bsrc, B["const"]], writes=[PB[bank]])
                        k.op("act", lambda e: e.copy(out=dstT[:, g4 * 512:(g4 + 1) * 512], in_=psbf(bank)[0:64, 0:512]), reads=[PB[bank]], writes=[BdstT])
                for g4 in range(4):
                    bk = 4 + g4

                    def mmkv(e):
                        for q4 in range(4):
                            c = 4 * g4 + q4
                            ins = e.matmul(ps[0:64, bk, q4 * 128:(q4 + 1) * 128], lhsT=rkd[:, c, :], rhs=rv[:, c, :], start=True, stop=True)
                        return ins
                    k.op("pe", mmkv, reads=[Brkd, Brv], writes=[PB[bk]])
                for c in range(NT - 1):
                    bk = 4 + c // 4
                    src = ps[0:64, bk, (c % 4) * 128:(c % 4 + 1) * 128]
                    z = c % 2
                    if c == 0:
                        k.op("dve", lambda e: e.tensor_copy(out=Sf[:, z, :], in_=src), reads=[PB[bk]], writes=[BSf[z]])
                    else:
                        k.op("dve", lambda e: e.scalar_tensor_tensor(out=Sf[:, z, :], in0=Sf[:, 1 - z, :], scalar=CHUNK_DECAY[h], in1=src,
                                                                     op0=ALU.mult, op1=ALU.add), reads=[PB[bk], BSf[1 - z]], writes=[BSf[z]])
                    k.op("act", lambda e: e.copy(out=Sbf[:, c, :], in_=Sf[:, z, :]), reads=[BSf[z]], writes=[BSbf])
                for g4 in range(4):
                    z = g4 % 2
                    yb = 6 + z
                    csl = slice(4 * g4, 4 * g4 + 4)
                    def st_step(q4):
                        c = 4 * g4 + q4
                        cs_ = slice(c * 128, (c + 1) * 128)
                        sbk = 4 + (c % 2)
                        k.op("pe", lambda e: e.matmul(ps[:, sbk, 0:128], lhsT=rkT[:, cs_], rhs=rqdT[:, cs_], start=True, stop=True),
                             reads=[BrkT, BrqdT], writes=[PB[sbk]])
                        k.op("dve", lambda e: e.tensor_tensor(out=inT[:, q4, :], in0=ps[:, sbk, 0:128], in1=mask2[:, h, :], op=ALU.mult),
                             reads=[PB[sbk], B["const"]], writes=[BinT[q4]])

                    def y_step(q4):
                        c = 4 * g4 + q4
                        cs_ = slice(c * 128, (c + 1) * 128)

                        def mmy(e):
                            ins = e.matmul(ps[:, yb, q4 * 128:(q4 + 1) * 128], lhsT=inT[:, q4, :], rhs=rv[:, c, :], start=True, stop=(c == 0))
                            if c > 0:
                                ins = e.matmul(ps[:, yb, q4 * 128:(q4 + 1) * 128], lhsT=rqdT[:, cs_], rhs=Sbf[:, c - 1, :], start=False, stop=True)
                            return ins
                        k.op("pe", mmy, reads=[BinT[q4], Brv, BrqdT, BSbf], writes=[PB[yb]])
                    st_step(0)
                    for q4 in range(4):
                        if q4 + 1 < 4:
                            st_step(q4 + 1)
                        y_step(q4)
                    Y = ps[:, yb, :].rearrange("p (c n) -> p c n", c=4)
                    S1, S2, M, MSQ, VAR, SD, RSTD, NB = (st[:, z, i, :] for i in range(8))
                    k.op("dve", lambda e: e.tensor_reduce(out=S1, in_=Y, axis=AX.X, op=ALU.add), reads=[PB[yb]], writes=[Bst[z]])
                    k.op("act", lambda e: e.activation(out=sqy[:].rearrange("p (c n) -> p c n", c=4), in_=Y, func=AF.Square), reads=[PB[yb]], writes=[Bsqy])
                    k.op("dve", lambda e: e.tensor_reduce(out=S2, in_=sqy[:].rearrange("p (c n) -> p c n", c=4), axis=AX.X, op=ALU.add),
                         reads=[Bsqy, Bst[z]], writes=[Bst[z]])
                    k.op("dve", lambda e: e.tensor_scalar(out=M, in0=S1, scalar1=1.0 / 128.0, scalar2=None, op0=ALU.mult), reads=[Bst[z]], writes=[Bst[z]])
                    k.op("dve", lambda e: e.tensor_tensor(out=MSQ, in0=M, in1=M, op=ALU.mult), reads=[Bst[z]], writes=[Bst[z]])
                    k.op("dve", lambda e: e.scalar_tensor_tensor(out=VAR, in0=S2, scalar=1.0 / 128.0, in1=MSQ, op0=ALU.mult, op1=ALU.subtract),
                         reads=[Bst[z]], writes=[Bst[z]])
                    k.op("act", lambda e: e.activation(out=SD, in_=VAR, func=AF.Sqrt, bias=float(EPS)), reads=[Bst[z]], writes=[Bst[z]])
                    k.op("dve", lambda e: e.reciprocal(out=RSTD, in_=SD), reads=[Bst[z]], writes=[Bst[z]])
                    k.op("dve", lambda e: e.scalar_tensor_tensor(out=NB, in0=M, scalar=-1.0, in1=RSTD, op0=ALU.mult, op1=ALU.mult),
                         reads=[Bst[z]], writes=[Bst[z]])
                    tyv = ty[:, z, :].rearrange("p (c n) -> p c n", c=4)
                    k.op("dve", lambda e: e.tensor_tensor(out=tyv, in0=Y, in1=bcast_last(RSTD, 128), op=ALU.mult), reads=[PB[yb], Bst[z]], writes=[Bty[z]])
                    k.op("dve", lambda e: e.tensor_tensor(out=tyv, in0=tyv, in1=bcast_last(NB, 128), op=ALU.add), reads=[Bty[z], Bst[z]], writes=[Bty[z]])
                    k.op("pool", lambda e: e.tensor_tensor(out=rett[:, csl, :], in0=tyv, in1=sgg[:, csl, :], op=ALU.mult),
                         reads=[Bty[z], Bsgg], writes=[Brett])
                for g4 in range(4):
                    bank = g4 % 2

                    def tr(e):
                        for q4 in range(4):
                            t = g4 * 4 + q4
                            ins = e.transpose(out=psbf(bank)[:, q4 * 128:(q4 + 1) * 128], in_=rett[:, t, :], identity=ident[:])
                        return ins
                    k.op("pe", tr, reads=[Brett, B["const"]], writes=[PB[bank]])
                    k.op("act", lambda e: e.copy(out=retT[:, h, g4 * 512:(g4 + 1) * 512], in_=psbf(bank)[:, 0:512]), reads=[PB[bank]], writes=[BretT])
            k.barrier()

    def ffn(l):
        moe = (l % 2 == 1)
        if moe:
            ctx_g = sb("gates", [128, NT, NEXP], F32)
            gates = ctx_g.__enter__()
            Bgates = k.buf("gates")
            router_logits(l, gates, Bgates)
            experts = [(mw1_d[e], mw3_d[e], mw2_d[e], DFE // 128) for e in range(NEXP)]
        else:
            mod_and_norm(l, "ffn")
            experts = [(w1_d, w3_d, w2_d, DFF // 128)]
        G = 4
        groups = []
        for ei, (a1, a3, a2, nch) in enumerate(experts):
            c0 = 0
            while c0 < nch:
                g = min(G, nch - c0)
                groups.append((ei, a1, a3, a2, c0, g))
                c0 += g
        with (sb("w13", [128, 2, 2, 8, G * 128], BF16) as w13, sb("w2s", [128, 2, G, D], BF16) as w2s,
              sb("gT", [128, 2, G, S], BF16) as gT, sb("sa", [128, 2, 512], BF16) as sa,
              sb("tmpf", [128, 2, D], F32) as tmpf):
            Bw13 = [k.buf("w13_0"), k.buf("w13_1")]
            Bw2 = [k.buf("w2_0"), k.buf("w2_1")]
            BgT = [k.buf("gT0"), k.buf("gT1")]
            Bsa = [k.buf("sa0"), k.buf("sa1")]
            Btf = [k.buf("tmpf0"), k.buf("tmpf1")]

            def load13(gi):
                ei, a1, a3, a2, c0, g = groups[gi]
                s = gi % 2
                k.dma("pool", w13[:, s, 0, :, 0:g * 128], a1[:, c0 * 128:(c0 + g) * 128].rearrange("(c p) n -> p c n", p=128), writes=[Bw13[s]])
                k.dma("pool", w13[:, s, 1, :, 0:g * 128], a3[:, c0 * 128:(c0 + g) * 128].rearrange("(c p) n -> p c n", p=128), writes=[Bw13[s]])

            def load2(gi):
                ei, a1, a3, a2, c0, g = groups[gi]
                s = gi % 2
                k.dma("pool", w2s[:, s, 0:g, :], a2[c0 * 128:(c0 + g) * 128, :].rearrange("(c p) n -> p c n", p=128), writes=[Bw2[s]])

            cnt = [0]

            def up(gi):
                ei, a1, a3, a2, c0, g = groups[gi]
                s = gi % 2
                for j in range(g):
                    for tb in range(4):
                        i2 = cnt[0] % 2
                        cnt[0] += 1
                        ba, bb = 2 * i2, 2 * i2 + 1

                        def mm(e):
                            for w, bk in ((0, ba), (1, bb)):
                                for kc in range(8):
                                    ins = e.matmul(ps[:, bk, :], lhsT=w13[:, s, w, kc, j * 128:(j + 1) * 128], rhs=hT[:, kc, tb * 512:(tb + 1) * 512],
                                                   start=(kc == 0), stop=(kc == 7))
                            return ins
                        k.op("pe", mm, reads=[Bw13[s], B["hT"]], writes=[PB[ba], PB[bb]])
                        k.op("act", lambda e: e.activation(out=sa[:, i2, :], in_=ps[:, ba, :], func=AF.Silu), reads=[PB[ba]], writes=[Bsa[i2]])
                        k.op("dve", lambda e: e.tensor_tensor(out=gT[:, s, j, tb * 512:(tb + 1) * 512], in0=ps[:, bb, :], in1=sa[:, i2, :], op=ALU.mult),
                             reads=[PB[bb], Bsa[i2]], writes=[BgT[s]])

            def down(gi):
                ei, a1, a3, a2, c0, g = groups[gi]
                s = gi % 2
                for t in range(NT):
                    b0 = 4 + 2 * (t % 2)

                    def mm(e):
                        for hf in range(2):
                            for j in range(g):
                                ins = e.matmul(ps[:, b0 + hf, :], lhsT=gT[:, s, j, t * 128:(t + 1) * 128], rhs=w2s[:, s, j, hf * 512:(hf + 1) * 512],
                                               start=(j == 0), stop=(j == g - 1))
                        return ins
                    k.op("pe", mm, reads=[BgT[s], Bw2[s]], writes=[PB[b0], PB[b0 + 1]])
                    sc = (gates[:, t, ei:ei + 1], Bgates) if moe else None
                    residual_update(t, None, (b0, b0 + 1), tmpf[:, t % 2, :], Btf[t % 2], scalar=sc)
                    if gi == len(groups) - 1 and (l, "ffn") == tuple(sublayers[-1]):
                        k.dma("sp", out_d[t * 128:(t + 1) * 128, :], x[:, t, :], reads=[XT[t]], writes=[out_buf])
                        stored.add(t)

            load13(0)
            load2(0)
            for gi in range(len(groups)):
                if gi + 1 < len(groups):
                    load13(gi + 1)
                up(gi)
                if gi > 0:
                    down(gi - 1)
                if gi + 1 < len(groups):
                    load2(gi + 1)
            down(len(groups) - 1)
            k.barrier()
        if moe:
            ctx_g.__exit__(None, None, None)

    def router_logits(l, gates, Bgates):
        with (sb("rt", [128, 8, NEXP], F32) as rt, sb("rhl", [128, 2, 8, NEXP], BF16) as rhl,
              sb("rtmp", [128, 8, NEXP], F32) as rtmp, sb("lo", [128, 2, D], BF16) as lo,
              sb("loT", [128, 2, 8, 128], BF16) as loT, sb("lg", [128, NT, NEXP], F32) as lg,
              sb("tk", [128, 8, NT, NEXP], F32) as tk, sb("tm", [128, 8, NT], F32) as tm):
            Brt, Blg, Btk = k.buf("rt"), k.buf("lg"), k.buf("tk")
            Blo = [k.buf("lo0"), k.buf("lo1")]
            BloT = [k.buf("loT0"), k.buf("loT1")]
            k.dma("sp", rt[:], router_d.rearrange("(c p) e -> p c e", p=128), writes=[Brt])
            k.op("dve", lambda e: e.tensor_copy(out=rhl[:, 0], in_=rt[:]), reads=[Brt], writes=[Brt])
            k.op("dve", lambda e: e.tensor_tensor(out=rtmp[:], in0=rt[:], in1=rhl[:, 0], op=ALU.subtract), reads=[Brt], writes=[Brt])
            k.op("dve", lambda e: e.tensor_copy(out=rhl[:, 1], in_=rtmp[:]), reads=[Brt], writes=[Brt])

            def per_tile(t, tmp, s2, Btmp, hbf, Bhbf):
                k.op("dve", lambda e: e.tensor_tensor(out=lo[:, s2, :], in0=tmp[:, s2, :], in1=hbf[:, s2, :], op=ALU.subtract),
                     reads=[Btmp, Bhbf], writes=[Blo[s2]])
                bank = 4 + s2

                def tr(e):
                    for kc in range(8):
                        ins = e.transpose(out=psbf(bank)[:, kc * 128:(kc + 1) * 128], in_=lo[:, s2, kc * 128:(kc + 1) * 128], identity=ident[:])
                    return ins
                k.op("pe", tr, reads=[Blo[s2], B["const"]], writes=[PB[bank]])
                k.op("act", lambda e: e.copy(out=loT[:, s2], in_=psbf(bank).rearrange("p (c n) -> p c n", c=8)), reads=[PB[bank]], writes=[BloT[s2]])
                lb = 6 + s2

                def mm(e):
                    n = 0
                    for (lh, rr) in ((0, 0), (0, 1), (1, 0)):
                        for kc in range(8):
                            lt = hT[:, kc, t * 128:(t + 1) * 128] if lh == 0 else loT[:, s2, kc, :]
                            ins = e.matmul(ps[:, lb, 0:NEXP], lhsT=lt, rhs=rhl[:, rr, kc, :], start=(n == 0), stop=(n == 23))
                            n += 1
                    return ins
                k.op("pe", mm, reads=[B["hT"], BloT[s2], Brt], writes=[PB[lb]])
                k.op("dve", lambda e: e.tensor_copy(out=lg[:, t, :], in_=ps[:, lb, 0:NEXP]), reads=[PB[lb]], writes=[Blg])
            mod_and_norm(l, "ffn", want_f32=per_tile)
            m1, m2, dd, w1, w2 = tm[:, 0], tm[:, 1], tm[:, 2], tm[:, 3], tm[:, 4]
            eq1, l2, eq2, g1, g2 = tk[:, 0], tk[:, 1], tk[:, 2], tk[:, 3], tk[:, 4]

            def dv(fn, reads=(Blg,), writes=(Btk,)):
                k.op("dve", fn, reads=list(reads) + [Btk], writes=list(writes))
            dv(lambda e: e.tensor_reduce(out=m1, in_=lg[:], axis=AX.X, op=ALU.max))
            dv(lambda e: e.tensor_tensor(out=eq1, in0=lg[:], in1=bcast_last(m1, NEXP), op=ALU.is_equal))
            dv(lambda e: e.scalar_tensor_tensor(out=l2, in0=eq1, scalar=-1e30, in1=lg[:], op0=ALU.mult, op1=ALU.add))
            dv(lambda e: e.tensor_reduce(out=m2, in_=l2, axis=AX.X, op=ALU.max))
            dv(lambda e: e.tensor_tensor(out=eq2, in0=l2, in1=bcast_last(m2, NEXP), op=ALU.is_equal))
            dv(lambda e: e.tensor_tensor(out=dd, in0=m1, in1=m2, op=ALU.subtract))
            k.op("act", lambda e: e.activation(out=w1, in_=dd, func=AF.Sigmoid), reads=[Btk], writes=[Btk])
            dv(lambda e: e.tensor_scalar(out=w2, in0=w1, scalar1=-1.0, scalar2=1.0, op0=ALU.mult, op1=ALU.add))
            dv(lambda e: e.tensor_tensor(out=g1, in0=eq1, in1=bcast_last(w1, NEXP), op=ALU.mult))
            dv(lambda e: e.tensor_tensor(out=g2, in0=eq2, in1=bcast_last(w2, NEXP), op=ALU.mult))
            k.op("dve", lambda e: e.tensor_tensor(out=gates[:], in0=g1, in1=g2, op=ALU.add), reads=[Btk], writes=[Bgates])
            k.barrier()

    stored = set()
    for (l, sub) in sublayers:
        if stop == "setup":
            break
        if sub == "mix":
            mod_and_norm(l, "mix")
            mixer(l)
        else:
            ffn(l)

    if len(stored) < NT:
        for t4 in range(4):
            k.dma("sp", out_d[512 * t4:512 * (t4 + 1), :].rearrange("(t p) d -> p t d", p=128), x[:, 4 * t4:4 * t4 + 4, :],
                  reads=XT[4 * t4:4 * t4 + 4], writes=[out_buf])
    E = k.E["sp"]
    E.h.wait_ge(out_buf.dsem, 16 * out_buf.dcount)
    nc.used_inputs = sorted(used)
    return nc


_CONSTS = None


def make_in_maps(inputs):
    global _CONSTS
    if _CONSTS is None:
        _CONSTS = host_consts()
    f = lambda a: np.ascontiguousarray(np.asarray(a, dtype=np.float32))
    shared = {
        "rel_bias_table": f(inputs["rel_bias_table"]),
        "norm_mix": f(inputs["norm_mix"]), "norm_ffn": f(inputs["norm_ffn"]),
        "w_mod": f(inputs["w_mod"]), "b_mod": f(inputs["b_mod"]), "w_in": f(inputs["w_in"]),
        "q_gain2": f(np.tile(np.asarray(inputs["q_gain"]), (1, 2)).T),
        "k_gain2": f(np.tile(np.asarray(inputs["k_gain"]), (1, 2)).T),
        "ret_gain": f(inputs["ret_gain"]), "w_out": f(inputs["w_out"]),
        "ffn_w1": f(np.asarray(inputs["ffn_w1"])[0]), "ffn_w3": f(np.asarray(inputs["ffn_w3"])[0]), "ffn_w2": f(np.asarray(inputs["ffn_w2"])[0]),
        "moe_router": f(np.asarray(inputs["moe_router"])[0]),
        "moe_w1": f(np.asarray(inputs["moe_w1"])[0]), "moe_w3": f(np.asarray(inputs["moe_w3"])[0]), "moe_w2": f(np.asarray(inputs["moe_w2"])[0]),
    }
    shared.update(_CONSTS)
    xs = np.asarray(inputs["x"], dtype=np.float32)
    cc = np.asarray(inputs["c"], dtype=np.float32)
    maps = []
    for b in range(8):
        m = dict(shared)
        m["x"] = np.ascontiguousarray(xs[b])
        m["cT"] = np.ascontiguousarray(cc[b].reshape(8, 128).T)
        maps.append(m)
    return maps


def kernel(**inputs):
    nc = build()
    maps = [{n: m[n] for n in nc.used_inputs} for m in make_in_maps(inputs)]
    res = run_bass_kernel_spmd(nc, maps, core_ids=list(range(8)))
    return np.stack([np.asarray(r["out"], dtype=np.float32) for r in res.results], axis=0)
```

```python
import contextlib
import math
import numpy as np
import concourse.bass as bass
import concourse.mybir as mybir
from concourse.bass_utils import run_bass_kernel_spmd

F32 = mybir.dt.float32
BF16 = mybir.dt.bfloat16
AF = mybir.ActivationFunctionType
ALU = mybir.AluOpType
AX = mybir.AxisListType

D = 1024
S = 2048
NT = 16
EPS = 1e-6
DFF = 2816
NEXP = 8
DFE = 3584
PATTERNS = ((128, 1), (512, 4), (2048, 16))


class Tick:
    __slots__ = ("sem", "val", "key")

    def __init__(self, sem, val, key):
        self.sem, self.val, self.key = sem, val, key


class Buf:
    def __init__(self, name, excl=False):
        self.name = name
        self.w = {}
        self.r = {}
        self.excl = excl
        self.dsem = None
        self.dcount = 0


class Eng:
    def __init__(self, name, h, sem):
        self.name, self.h, self.sem = name, h, sem
        self.count = 0
        self.seen = {}


class K:
    def __init__(self, nc):
        self.nc = nc
        self.E = {}
        for name, h in (("pe", nc.tensor), ("act", nc.scalar), ("dve", nc.vector),
                        ("pool", nc.gpsimd), ("sp", nc.sync)):
            self.E[name] = Eng(name, h, nc.alloc_semaphore("sem_" + name))
        self.dbufs = []
        self.nbuf = 0

    def buf(self, name, excl=False):
        self.nbuf += 1
        return Buf("%s_%d" % (name, self.nbuf), excl)

    def _deps(self, reads, writes):
        deps = {}

        def add(t):
            o = deps.get(t.key)
            if o is None or o.val < t.val:
                deps[t.key] = t
        for b in reads:
            for t in b.w.values():
                add(t)
            if b.excl:
                for t in b.r.values():
                    add(t)
        for b in writes:
            for t in b.w.values():
                add(t)
            for t in b.r.values():
                add(t)
        return deps

    def _wait(self, E, deps):
        for key, t in deps.items():
            if key == "pe" and E.name == "pe":
                continue
            if E.seen.get(key, 0) >= t.val:
                continue
            E.h.wait_ge(t.sem, t.val)
            E.seen[key] = t.val

    def _mark(self, t, reads, writes):
        for b in reads:
            o = b.r.get(t.key)
            if o is None or o.val < t.val:
                b.r[t.key] = t
        for b in writes:
            b.w = {t.key: t}
            b.r = {}

    def op(self, en, fn, reads=(), writes=()):
        E = self.E[en]
        self._wait(E, self._deps(reads, writes))
        ins = fn(E.h)
        E.count += 1
        ins.then_inc(E.sem, 1)
        t = Tick(E.sem, E.count, E.name)
        self._mark(t, reads, writes)
        return t

    def dma(self, q, out, in_, reads=(), writes=()):
        E = self.E[q]
        self._wait(E, self._deps(reads, writes))
        dst = writes[0]
        if dst.dsem is None:
            dst.dsem = self.nc.alloc_semaphore("d_" + dst.name)
            self.dbufs.append(dst)
        dst.dcount += 1
        if hasattr(in_, "_ap"):
            in_ = in_._ap()
        E.h.dma_start(out=out, in_=in_).then_inc(dst.dsem, 16)
        t = Tick(dst.dsem, 16 * dst.dcount, "dma:" + dst.name)
        for b in reads:
            b.r[t.key] = t
        for b in writes:
            b.w = {k: v for k, v in b.w.items() if k == t.key}
            b.w[t.key] = t
            b.r = {}
        return t

    def barrier(self):
        ticks = {}
        for E in self.E.values():
            if E.count > 0:
                ticks[E.name] = Tick(E.sem, E.count, E.name)
        for b in self.dbufs:
            ticks["dma:" + b.name] = Tick(b.dsem, 16 * b.dcount, "dma:" + b.name)
        for E in self.E.values():
            for key, t in ticks.items():
                if E.seen.get(key, 0) >= t.val:
                    continue
                E.h.wait_ge(t.sem, t.val)
                E.seen[key] = t.val


def bcast_last(ap, n):
    return bass.AP(ap.tensor, ap.offset, [list(d) for d in ap.ap] + [[0, n]])


def bcast_mid(ap, n):
    d = [list(x) for x in ap.ap]
    return bass.AP(ap.tensor, ap.offset, [d[0], [0, n]] + d[1:])


def _t5_bucket(n):
    n = np.maximum(n, 0)
    nf = np.maximum(n.astype(np.float32), np.float32(16.0))
    large = 16 + (np.log(nf / np.float32(16.0)) / np.float32(math.log(2048 / 16)) * np.float32(16)).astype(np.int32)
    large = np.minimum(large, 31)
    return np.where(n < 16, n, large)


def host_consts():
    c = {}
    c["c_ident"] = np.eye(128, dtype=np.float32)
    blk = np.zeros((128, 128), np.float32)
    blk[:64, :64] = 1
    blk[64:, 64:] = 1
    c["c_blk"] = blk
    half = 32
    inv = (np.float32(10000.0) ** (-np.arange(half, dtype=np.float32) / np.float32(half))).astype(np.float32)
    pos = np.arange(S, dtype=np.float32)
    ang = (pos[:, None] * inv[None, :]).astype(np.float32)
    cs = np.concatenate([np.cos(ang), np.sin(ang)], axis=1).astype(np.float32)
    c["c_cs"] = np.ascontiguousarray(cs.reshape(NT, 128, 64).transpose(1, 0, 2))
    lg = np.log(1.0 - 2.0 ** (-5.0 - np.arange(4, dtype=np.float64)))
    idx = np.arange(128, dtype=np.float64)
    qdec = np.exp((idx[:, None] + 1.0) * lg[None, :]) * 0.125
    kdec = np.exp((127.0 - idx[:, None]) * lg[None, :])
    c["c_dec"] = np.concatenate([qdec, kdec], axis=1).astype(np.float32)
    kk = idx[:, None, None]
    qq = idx[None, None, :]
    m2 = np.exp(-(kk + 1.0) * lg[None, :, None]) * (qq >= kk)
    c["c_mask2"] = m2.astype(np.float32)
    oh = np.zeros((33, 3, 384), np.float32)
    for b, (_, d) in enumerate(PATTERNS):
        m = np.arange(384)
        rel = m - 127
        ok = (rel >= 0) & (rel <= 128)
        bk = _t5_bucket(rel * d)
        for mm in range(384):
            if ok[mm]:
                oh[bk[mm], b, mm] = 1.0
            else:
                oh[32, b, mm] = 1.0
    c["c_oh"] = oh
    return c


CHUNK_DECAY = [float((1.0 - 2.0 ** (-5.0 - h)) ** 128) for h in range(4)]


def build(sublayers=((0, "mix"), (0, "ffn"), (1, "mix"), (1, "ffn")), stop=None):
    nc = bass.Bass("TRN2", target_bir_lowering=False)
    k = K(nc)

    uid = [0]

    def sb(name, shape, dt):
        uid[0] += 1
        return nc.sbuf_tensor("s%d_%s" % (uid[0], name), shape, dt)

    def sba(name, shape, dt):
        uid[0] += 1
        return nc.alloc_sbuf_tensor("s%d_%s" % (uid[0], name), shape, dt)

    shapes = {
        "x": (S, D), "cT": (128, 8), "rel_bias_table": (32, 8), "norm_mix": (2, D), "norm_ffn": (2, D),
        "w_mod": (2, D, 6 * D), "b_mod": (2, 6 * D), "w_in": (2, D, 3072), "q_gain2": (128, 2), "k_gain2": (128, 2),
        "ret_gain": (2, 512), "w_out": (2, D, D), "ffn_w1": (D, DFF), "ffn_w3": (D, DFF), "ffn_w2": (DFF, D),
        "moe_router": (D, NEXP), "moe_w1": (NEXP, D, DFE), "moe_w3": (NEXP, D, DFE), "moe_w2": (NEXP, DFE, D),
        "c_ident": (128, 128), "c_blk": (128, 128), "c_cs": (128, NT, 64), "c_dec": (128, 8),
        "c_mask2": (128, 4, 128), "c_oh": (33, 3, 384),
    }
    used = {}

    class Lazy:
        def __init__(self, name):
            self.name = name

        def _ap(self):
            if self.name not in used:
                used[self.name] = nc.dram_tensor(self.name, list(shapes[self.name]), F32, kind="ExternalInput").ap()
            return used[self.name]

        def __getitem__(self, key):
            return self._ap()[key]

        def rearrange(self, *a, **kw):
            return self._ap().rearrange(*a, **kw)

        @property
        def tensor(self):
            return self._ap().tensor

        def ap(self):
            return self._ap()

    x_d, cT_d, table_d, nmix_d, nffn_d = Lazy("x"), Lazy("cT"), Lazy("rel_bias_table"), Lazy("norm_mix"), Lazy("norm_ffn")
    wmod_d, bmod_d, win_d, qg_d, kg_d = Lazy("w_mod"), Lazy("b_mod"), Lazy("w_in"), Lazy("q_gain2"), Lazy("k_gain2")
    rgain_d, wout_d, w1_d, w3_d, w2_d = Lazy("ret_gain"), Lazy("w_out"), Lazy("ffn_w1"), Lazy("ffn_w3"), Lazy("ffn_w2")
    router_d, mw1_d, mw3_d, mw2_d = Lazy("moe_router"), Lazy("moe_w1"), Lazy("moe_w3"), Lazy("moe_w2")
    cid_d, cblk_d, ccs_d, cdec_d, cm2_d, coh_d = (Lazy("c_ident"), Lazy("c_blk"), Lazy("c_cs"), Lazy("c_dec"),
                                                  Lazy("c_mask2"), Lazy("c_oh"))
    out_d = nc.dram_tensor("out", [S, D], F32, kind="ExternalOutput").ap()
    ebscr = nc.dram_tensor("ebscr", [24, 128, 384], F32, kind="Internal")
    ebscr_buf = k.buf("ebscr")
    out_buf = k.buf("outd")

    x = sba("x", [128, NT, D], F32)
    hT = sba("hT", [128, 8, S], BF16)
    ident = sba("ident", [128, 128], BF16)
    blk = sba("blk", [128, 128], BF16)
    ones = sba("ones", [128, 128], BF16)
    onesf = sba("onesf", [1, 128], F32)
    cs = sba("cs", [128, NT, 64], F32)
    dec = sba("dec", [128, 8], F32)
    mask2 = sba("mask2", [128, 4, 128], F32)
    cact = sba("cact", [128, 8], F32)
    cact_rep = sba("cact_rep", [128, 8, 128], F32)
    gains = sba("gains", [128, 4], F32)
    gate = sba("gate", [128, D], F32)
    ss = sba("ss", [128, NT], F32)
    rs = sba("rs", [128, NT], F32)
    ps = nc.alloc_psum_tensor("ps", [128, 8, 512], F32)
    PB = [k.buf("psb%d" % i, excl=True) for i in range(8)]

    B = {n: k.buf(n) for n in ("x", "hT", "const", "cact", "gains", "gate", "ss", "rs")}
    XT = [k.buf("xt%d" % t) for t in range(NT)]

    def psbf(i):
        return ps[:, i, :].bitcast(BF16)

    for t4 in range(4):
        k.dma("sp", x[:, 4 * t4:4 * t4 + 4, :],
              x_d[512 * t4:512 * (t4 + 1), :].rearrange("(t p) d -> p t d", p=128),
              writes=[B["x"]])
    Bcp = k.buf("constp")
    k.dma("pool", ident[:], cid_d, writes=[Bcp])
    k.dma("pool", blk[:], cblk_d, writes=[Bcp])
    k.dma("sp", cs[:], ccs_d, writes=[B["const"]])
    k.dma("sp", dec[:], cdec_d, writes=[B["const"]])
    k.dma("sp", mask2[:], cm2_d, writes=[B["const"]])
    k.dma("sp", cact[:], cT_d, writes=[B["cact"]])
    k.dma("sp", gains[:, 0:2], qg_d, writes=[B["gains"]])
    k.dma("sp", gains[:, 2:4], kg_d, writes=[B["gains"]])
    k.op("dve", lambda e: e.memset(ones[:], 1.0), reads=[Bcp], writes=[B["const"]])
    k.op("dve", lambda e: e.memset(onesf[:], 1.0), writes=[B["const"]])
    k.op("dve", lambda e: e.tensor_scalar(out=gains[:], in0=gains[:], scalar1=8.0, scalar2=None, op0=ALU.mult),
         reads=[B["gains"]], writes=[B["gains"]])
    k.op("act", lambda e: e.activation(out=cact[:], in_=cact[:], func=AF.Silu), reads=[B["cact"]], writes=[B["cact"]])
    for kc in range(8):
        k.op("dve", lambda e, kc=kc: e.tensor_copy(out=cact_rep[:, kc, :], in_=cact[:, kc:kc + 1].to_broadcast([128, 128])),
             reads=[B["cact"]], writes=[B["const"]])
    for XB in XT:
        XB.w = dict(B["x"].w)

    eb_pending = [any(sl == "mix" for _, sl in sublayers)]

    def dump(name, t, dt, bufs):
        dd = nc.dram_tensor("dbg_" + name, list(t.shape), dt, kind="ExternalOutput").ap()
        k.dma("sp", dd, t[:], reads=bufs, writes=[out_buf])

    def mod_and_norm(l, which, want_f32=None):
        base = 0 if which == "mix" else 3
        NS = 3 if (want_f32 is None and not eb_pending[0]) else 2
        norm_d = nmix_d if which == "mix" else nffn_d
        with (sb("A", [128, D], F32) as A, sb("shift", [128, D], F32) as shift,
              sb("normB", [128, D], F32) as normB, sb("wm", [128, NS, 8, 512], F32) as wm,
              sb("bm", [1, 3 * D], F32) as bm, sb("junk", [128, D], BF16) as junk,
              sb("tmp", [128, 2, D], F32) as tmp, sb("hbf", [128, 2, D], BF16) as hbf,
              sb("sq", [128, NT], F32) as sqv):
            BA, Bs, Bn, Bbm, Bj = k.buf("A"), k.buf("shift"), k.buf("normB"), k.buf("bm"), k.buf("junk")
            Bwm = [k.buf("wm%d" % q) for q in range(NS)]
            Btmp = [k.buf("tmp0"), k.buf("tmp1")]
            Bhbf = [k.buf("hbf0"), k.buf("hbf1")]
            k.dma("sp", normB[:], bass.AP(norm_d.tensor, l * D, [[0, 128], [1, D]]), writes=[Bn])
            k.dma("sp", bm[:], bmod_d[l:l + 1, base * D:(base + 3) * D], writes=[Bbm])
            k.op("dve", lambda e: e.tensor_scalar(out=normB[:], in0=normB[:], scalar1=32.0, scalar2=None, op0=ALU.mult),
                 reads=[Bn], writes=[Bn])
            for t in range(NT):
                k.op("act", lambda e: e.activation(out=junk[:], in_=x[:, t, :], func=AF.Square, accum_out=ss[:, t:t + 1]),
                     reads=[XT[t]], writes=[Bj, B["ss"]])
            k.op("act", lambda e: e.activation(out=sqv[:], in_=ss[:], func=AF.Sqrt, bias=float(D * EPS)), reads=[B["ss"]], writes=[B["rs"]])
            k.op("dve", lambda e: e.reciprocal(out=rs[:], in_=sqv[:]), reads=[B["rs"]], writes=[B["rs"]])
            eb_steps = []
            es = contextlib.ExitStack()
            if eb_pending[0]:
                eb_pending[0] = False
                tab = es.enter_context(sb("tab", [33, 8], F32))
                tabb = es.enter_context(sb("tabb", [33, 8, 128], F32))
                oh = es.enter_context(sb("oh", [33, 3, 384], F32))
                ebst = es.enter_context(sb("ebst", [128, 4, 384], F32))
                Bt = k.buf("tab")
                Bst = [k.buf("ebst%d" % q) for q in range(4)]
                k.op("dve", lambda e: e.memset(tab[:], -30000.0), writes=[Bt])
                k.dma("sp", tab[0:32, :], table_d, writes=[Bt])
                k.dma("sp", oh[:], coh_d, writes=[Bt])
                k.op("dve", lambda e: e.tensor_copy(out=tabb[:], in_=bcast_last(tab[:], 128)), reads=[Bt], writes=[Bt])
                for i_ in range(24):
                    def step(i_=i_):
                        b_, h_ = i_ // 8, i_ % 8
                        bank_ = 4 + (i_ % 2)
                        k.op("pe", lambda e: e.matmul(ps[:, bank_, 0:384], lhsT=tabb[:, h_, :], rhs=oh[:, b_, :], start=True, stop=True),
                             reads=[Bt], writes=[PB[bank_]])
                        k.op("act", lambda e: e.activation(out=ebst[:, i_ % 4, :], in_=ps[:, bank_, 0:384], func=AF.Exp),
                             reads=[PB[bank_]], writes=[Bst[i_ % 4]])
                        k.dma("pool", bass.AP(ebscr, i_ * 128 * 384, [[384, 128], [1, 384]]),
                              ebst[:, i_ % 4, :], reads=[Bst[i_ % 4]], writes=[ebscr_buf])
                    eb_steps.append(step)
            for nb in range(6):
                slot = nb % NS
                col0 = base * D + nb * 512
                k.dma("sp", wm[:, slot], wmod_d[l, :, col0:col0 + 512].rearrange("(c p) n -> p c n", p=128), writes=[Bwm[slot]])
                bank = nb % 2

                def mm(e):
                    for kc in range(8):
                        e.matmul(ps[:, bank, :], lhsT=cact_rep[:, kc, :], rhs=wm[:, slot, kc, :], start=(kc == 0), stop=False)
                    return e.matmul(ps[:, bank, :], lhsT=onesf[0:1, :], rhs=bm[0:1, nb * 512:(nb + 1) * 512], start=False, stop=True)
                k.op("pe", mm, reads=[Bwm[slot], Bbm, B["const"]], writes=[PB[bank]])
                half = slice((nb % 2) * 512, (nb % 2) * 512 + 512)
                kind = nb // 2
                if kind == 0:
                    k.op("act", lambda e: e.copy(out=shift[:, half], in_=ps[:, bank, :]), reads=[PB[bank]], writes=[Bs])
                elif kind == 1:
                    k.op("dve", lambda e: e.scalar_tensor_tensor(out=A[:, half], in0=ps[:, bank, :], scalar=1.0, in1=normB[:, half],
                                                                 op0=ALU.add, op1=ALU.mult), reads=[PB[bank], Bn], writes=[BA])
                else:
                    k.op("act", lambda e: e.copy(out=gate[:, half], in_=ps[:, bank, :]), reads=[PB[bank]], writes=[B["gate"]])
                for _ in range(4):
                    if eb_steps:
                        eb_steps.pop(0)()
            while eb_steps:
                eb_steps.pop(0)()
            es.close()
            for t in range(NT):
                s2 = t % 2
                k.op("dve", lambda e: e.scalar_tensor_tensor(out=tmp[:, s2, :], in0=x[:, t, :], scalar=rs[:, t:t + 1], in1=A[:],
                                                             op0=ALU.mult, op1=ALU.mult), reads=[XT[t], B["rs"], BA], writes=[Btmp[s2]])
                if want_f32 is not None:
                    k.op("dve", lambda e: e.tensor_tensor(out=tmp[:, s2, :], in0=tmp[:, s2, :], in1=shift[:], op=ALU.add),
                         reads=[Btmp[s2], Bs], writes=[Btmp[s2]])
                    k.op("act", lambda e: e.copy(out=hbf[:, s2, :], in_=tmp[:, s2, :]), reads=[Btmp[s2]], writes=[Bhbf[s2]])
                else:
                    k.op("dve", lambda e: e.tensor_tensor(out=hbf[:, s2, :], in0=tmp[:, s2, :], in1=shift[:], op=ALU.add),
                         reads=[Btmp[s2], Bs], writes=[Bhbf[s2]])
                bank = 2 + (t % 2)

                def tr(e):
                    for kc in range(8):
                        ins = e.transpose(out=psbf(bank)[:, kc * 128:(kc + 1) * 128], in_=hbf[:, s2, kc * 128:(kc + 1) * 128], identity=ident[:])
                    return ins
                k.op("pe", tr, reads=[Bhbf[s2], B["const"]], writes=[PB[bank]])
                k.op("act", lambda e: e.copy(out=hT[:, :, t * 128:(t + 1) * 128], in_=psbf(bank).rearrange("p (c n) -> p c n", c=8)),
                     reads=[PB[bank]], writes=[B["hT"]])
                if want_f32 is not None:
                    want_f32(t, tmp, s2, Btmp[s2], hbf, Bhbf[s2])
            k.barrier()

    def residual_update(t, psrc, banks, tmpt, Btmpt, scalar=None):
        src = ps[:, banks[0]:banks[0] + 2, :].rearrange("p a n -> p (a n)")
        if scalar is None:
            k.op("dve", lambda e: e.tensor_tensor(out=tmpt, in0=src, in1=gate[:], op=ALU.mult),
                 reads=[PB[banks[0]], PB[banks[1]], B["gate"]], writes=[Btmpt])
        else:
            k.op("dve", lambda e: e.scalar_tensor_tensor(out=tmpt, in0=src, scalar=scalar[0], in1=gate[:], op0=ALU.mult, op1=ALU.mult),
                 reads=[PB[banks[0]], PB[banks[1]], B["gate"], scalar[1]], writes=[Btmpt])
        k.op("pool", lambda e: e.tensor_tensor(out=x[:, t, :], in0=x[:, t, :], in1=tmpt, op=ALU.add),
             reads=[Btmpt, XT[t]], writes=[XT[t]])

    def mixer(l):
        with sb("attnT", [128, 4, S], BF16) as attnT:
            BattnT = k.buf("attnT")
            if stop == "modnorm":
                dump("hT", hT, BF16, [B["hT"]])
                dump("gate", gate, F32, [B["gate"]])
                return
            attention(l, attnT, BattnT)
            if stop == "attn_proj" or (stop and stop.startswith("attn_units:")):
                return
            if stop == "attn":
                dump("attnT", attnT, BF16, [BattnT])
                return
            with sb("retT", [128, 4, S], BF16) as retT:
                BretT = k.buf("retT")
                retention(l, retT, BretT)
                if stop == "ret":
                    dump("retT", retT, BF16, [BretT])
                    return
                with (sb("wo", [128, 8, D], BF16) as wo, sb("tmpo", [128, 4, D], F32) as tmpo):
                    Bwo = k.buf("wo")
                    Bt2 = [k.buf("tmpo%d" % q) for q in range(4)]
                    for hf in range(2):
                        k.dma("pool", wo[:, :, hf * 512:(hf + 1) * 512],
                              wout_d[l, :, hf * 512:(hf + 1) * 512].rearrange("(c p) n -> p c n", p=128), writes=[Bwo])
                    for t in range(NT):
                        b0 = 2 * (t % 4)

                        def mm(e):
                            for hf in range(2):
                                for kc in range(8):
                                    src = attnT if kc < 4 else retT
                                    ins = e.matmul(ps[:, b0 + hf, :], lhsT=src[:, kc % 4, t * 128:(t + 1) * 128], rhs=wo[:, kc, hf * 512:(hf + 1) * 512],
                                                   start=(kc == 0), stop=(kc == 7))
                            return ins
                        k.op("pe", mm, reads=[BattnT, BretT, Bwo], writes=[PB[b0], PB[b0 + 1]])
                        residual_update(t, None, (b0, b0 + 1), tmpo[:, t % 4, :], Bt2[t % 4])
                    k.barrier()

    def attention(l, mixT, BmixT):
        with (sb("wqkv", [128, 2, 8, 3, 128], BF16) as wqkv, sb("qT", [128, S], BF16) as qT,
              sb("kT", [128, S], BF16) as kT, sb("V", [128, 3, 16, 128], BF16) as V,
              sb("EB", [128, 3, 2, 256], BF16) as EB, sb("acc", [128, 2, S], F32) as acc,
              sb("esb", [128, 3, 2, 256], BF16) as esb, sb("psb", [128, 3, 2, 256], BF16) as psb,
              sb("sqb", [128, 2, 512], BF16) as sqb, sb("rsb", [128, 2, 512], F32) as rsb,
              sb("vT", [128, S], BF16) as vT):
            Bw = [k.buf("wqkv0"), k.buf("wqkv1")]
            BqT, BkT, BV, BEB, Bacc = k.buf("qT"), k.buf("kT"), k.buf("V"), k.buf("EB"), k.buf("acc")
            Besb = [k.buf("esb%d" % q) for q in range(3)]
            Bpsb = [k.buf("psb%d" % q) for q in range(3)]
            Bsq = [k.buf("sqb0"), k.buf("sqb1")]
            Brs = [k.buf("rsb0"), k.buf("rsb1")]
            BvT = k.buf("vT")

            def load_w(c):
                s = c % 2
                for j in range(3):
                    col = j * 512 + c * 128
                    k.dma("pool", wqkv[:, s, :, j, :], win_d[l, :, col:col + 128].rearrange("(c p) n -> p c n", p=128), writes=[Bw[s]])
            load_w(0)
            for c in range(4):
                s = c % 2
                if c + 1 < 4:
                    load_w(c + 1)
                for b in range(3):
                    for hh in range(2):
                        i = b * 8 + c * 2 + hh
                        k.dma("pool", EB[:, b, hh, :], bass.AP(ebscr, i * 128 * 384 + 127, [[383, 128], [1, 256]]),
                              reads=[ebscr_buf], writes=[BEB])
                qk_steps, vp_steps, vt_steps = [], [], []
                ci = 0
                for j, (dst, Bdst) in enumerate(((qT, BqT), (kT, BkT))):
                    gcol = gains[:, 2 * j + l:2 * j + l + 1]
                    for tb in range(4):
                        z = ci % 2
                        ci += 1

                        def qk(j=j, dst=dst, Bdst=Bdst, gcol=gcol, tb=tb, z=z):
                            ba, bb = 2 * z, 2 * z + 1

                            def mm(e):
                                for kc in range(8):
                                    ins = e.matmul(ps[:, ba, :], lhsT=wqkv[:, s, kc, j, :], rhs=hT[:, kc, tb * 512:(tb + 1) * 512],
                                                   start=(kc == 0), stop=(kc == 7))
                                return ins
                            k.op("pe", mm, reads=[Bw[s], B["hT"]], writes=[PB[ba]])
                            k.op("act", lambda e: e.activation(out=sqb[:, z, :], in_=ps[:, ba, :], func=AF.Square), reads=[PB[ba]], writes=[Bsq[z]])
                            k.op("pe", lambda e: e.matmul(ps[:, bb, :], lhsT=blk[:], rhs=sqb[:, z, :], start=True, stop=True),
                                 reads=[Bsq[z], B["const"]], writes=[PB[bb]])
                            k.op("act", lambda e: e.activation(out=rsb[:, z, :], in_=ps[:, bb, :], func=AF.Sqrt, bias=float(64 * EPS)),
                                 reads=[PB[bb]], writes=[Brs[z]])
                            k.op("dve", lambda e: e.reciprocal(out=rsb[:, z, :], in_=rsb[:, z, :]), reads=[Brs[z]], writes=[Brs[z]])
                            k.op("dve", lambda e: e.scalar_tensor_tensor(out=dst[:, tb * 512:(tb + 1) * 512], in0=ps[:, ba, :], scalar=gcol,
                                                                         in1=rsb[:, z, :], op0=ALU.mult, op1=ALU.mult),
                                 reads=[PB[ba], Brs[z], B["gains"]], writes=[Bdst])
                        qk_steps.append(qk)
                for tb in range(4):
                    def vp(tb=tb):
                        bk = 4 + (tb % 2)

                        def mm(e):
                            for kc in range(8):
                                ins = e.matmul(ps[:, bk, :], lhsT=wqkv[:, s, kc, 2, :], rhs=hT[:, kc, tb * 512:(tb + 1) * 512],
                                               start=(kc == 0), stop=(kc == 7))
                            return ins
                        k.op("pe", mm, reads=[Bw[s], B["hT"]], writes=[PB[bk]])
                        k.op("act", lambda e: e.copy(out=vT[:, tb * 512:(tb + 1) * 512], in_=ps[:, bk, :]), reads=[PB[bk]], writes=[BvT])
                    vp_steps.append(vp)
                gi = 0
                for b, (_, d) in enumerate(PATTERNS):
                    L = S // d
                    nb = max(L // 128, 1)
                    for g4 in range(4):
                        def vt(b=b, d=d, nb=nb, g4=g4, bk=6 + (gi % 2)):
                            def tr(e):
                                for q4 in range(4):
                                    bi = g4 * 4 + q4
                                    r, n = bi // nb, bi % nb
                                    t0 = r + d * 128 * n
                                    ins = e.transpose(out=psbf(bk)[:, q4 * 128:(q4 + 1) * 128], in_=vT[:, t0:t0 + 127 * d + 1:d], identity=ident[:])
                                return ins
                            k.op("pe", tr, reads=[BvT, B["const"]], writes=[PB[bk]])
                            k.op("act", lambda e: e.copy(out=V[:, b, g4 * 4:(g4 + 1) * 4, :], in_=psbf(bk)[:, 0:512].rearrange("p (a n) -> p a n", a=4)),
                                 reads=[PB[bk]], writes=[BV])
                        gi += 1
                        vt_steps.append(vt)
                for i4 in range(4):
                    qk_steps[i4]()
                    vp_steps[i4]()
                for i4 in range(4):
                    qk_steps[4 + i4]()
                    for vtt in vt_steps[3 * i4:3 * i4 + 3]:
                        vtt()
                if stop == "attn_proj":
                    dump("qT", qT, BF16, [BqT]); dump("kT", kT, BF16, [BkT]); dump("V", V, BF16, [BV]); dump("EB", EB, BF16, [BEB])
                    return
                units = []
                for b, (_, d) in enumerate(PATTERNS):
                    L = S // d
                    nb = max(L // 128, 1)
                    for r in range(d):
                        for n in range(nb):
                            units.append((b, d, nb, r, n))

                def scores(u, ui):
                    b, d, nb, r, n = u
                    sb = 2 + 2 * (ui % 2)
                    tq = r + d * 128 * n
                    qs = slice(tq, tq + 127 * d + 1, d)
                    psv = ps[:, sb:sb + 2, 0:256]

                    def mm(e):
                        for hh in range(2):
                            pr = slice(hh * 64, hh * 64 + 64)
                            ins = e.matmul(psv[:, hh, 0:128], lhsT=kT[pr, qs], rhs=qT[pr, qs], start=True, stop=True)
                            if n > 0:
                                tp = r + d * 128 * (n - 1)
                                ins = e.matmul(psv[:, hh, 128:256], lhsT=kT[pr, tp:tp + 127 * d + 1:d], rhs=qT[pr, qs], start=True, stop=True)
                        return ins
                    k.op("pe", mm, reads=[BqT, BkT], writes=[PB[sb], PB[sb + 1]])
                    w = 256 if n > 0 else 128
                    k.op("act", lambda e: e.activation(out=esb[:, ui % 3, :, 0:w], in_=psv[:, :, 0:w], func=AF.Exp, scale=0.125),
                         reads=[PB[sb], PB[sb + 1]], writes=[Besb[ui % 3]])
                    k.op("dve", lambda e: e.tensor_tensor(out=psb[:, ui % 3, :, 0:w], in0=esb[:, ui % 3, :, 0:w], in1=EB[:, b, :, 0:w], op=ALU.mult),
                         reads=[Besb[ui % 3], BEB], writes=[Bpsb[ui % 3]])

                def pv(u, ui):
                    b, d, nb, r, n = u
                    ob = 6 + (ui % 2)
                    pov = ps[:, ob, 0:256].rearrange("p (a n) -> p a n", a=2)
                    bi = r * nb + n

                    def mm(e):
                        for hh in range(2):
                            pr = slice(hh * 64, hh * 64 + 64)
                            for a in range(2):
                                lc = V[:, b, bi, pr] if a == 0 else ones[:, 0:64]
                                ins = e.matmul(pov[pr, a, :], lhsT=lc, rhs=psb[:, ui % 3, hh, 0:128], start=True, stop=(n == 0))
                                if n > 0:
                                    lp = V[:, b, bi - 1, pr] if a == 0 else ones[:, 0:64]
                                    ins = e.matmul(pov[pr, a, :], lhsT=lp, rhs=psb[:, ui % 3, hh, 128:256], start=False, stop=True)
                        return ins
                    k.op("pe", mm, reads=[Bpsb[ui % 3], BV, B["const"]], writes=[PB[ob]])
                    tq = r + d * 128 * n
                    dst = acc[:, :, tq:tq + 127 * d + 1:d]
                    if b == 0:
                        k.op("dve", lambda e: e.tensor_copy(out=dst, in_=pov), reads=[PB[ob]], writes=[Bacc])
                    else:
                        k.op("dve", lambda e: e.tensor_tensor(out=dst, in0=dst, in1=pov, op=ALU.add), reads=[PB[ob], Bacc], writes=[Bacc])
                if stop and stop.startswith("attn_units:"):
                    units = units[:int(stop.split(":")[1])]
                    k.op("dve", lambda e: e.memset(acc[:], 0.0), writes=[Bacc])
                nopv = bool(stop and stop.endswith(":nopv"))
                LAG = 2
                for ui, u in enumerate(units):
                    scores(u, ui)
                    if ui >= LAG and not nopv:
                        pv(units[ui - LAG], ui - LAG)
                if units and not nopv:
                    for uj in range(max(len(units) - LAG, 0), len(units)):
                        pv(units[uj], uj)
                if stop and stop.startswith("attn_units:"):
                    dump("acc", acc, F32, [Bacc]); dump("esb", esb, BF16, Besb); dump("psb", psb, BF16, Bpsb)
                    return
                k.op("dve", lambda e: e.reciprocal(out=acc[:, 1, :], in_=acc[:, 1, :]), reads=[Bacc], writes=[Bacc])
                k.op("dve", lambda e: e.tensor_tensor(out=mixT[:, c, :], in0=acc[:, 0, :], in1=acc[:, 1, :], op=ALU.mult),
                     reads=[Bacc], writes=[BmixT])
            k.barrier()

    def retention(l, retT, BretT):
        with (sb("wr", [128, 2, 8, 384], BF16) as wr, sb("rqd", [128, NT, 64], BF16) as rqd,
              sb("rk", [128, NT, 64], BF16) as rk, sb("rkd", [128, NT, 64], BF16) as rkd,
              sb("rqdT", [64, S], BF16) as rqdT, sb("rkT", [64, S], BF16) as rkT,
              sb("rv", [128, NT, 128], BF16) as rv, sb("sgg", [128, NT, 128], BF16) as sgg,
              sb("rett", [128, NT, 128], BF16) as rett, sb("gB", [128, 512], F32) as gB,
              sb("rot", [128, 6, 4, 2, 32], F32) as rot, sb("sgt", [128, 2, 2, 128], F32) as sgt,
              sb("Sf", [64, 2, 128], F32) as Sf, sb("Sbf", [64, NT, 128], BF16) as Sbf,
              sb("inT", [128, 4, 128], BF16) as inT, sb("sqy", [128, 512], F32) as sqy,
              sb("ty", [128, 2, 512], F32) as ty, sb("st", [128, 2, 8, 4], F32) as st):
            Bw = [k.buf("wr0"), k.buf("wr1")]
            Brqd, Brk, Brkd, BrqdT, BrkT, Brv, Bsgg, Brett, BgB, Brot, Bsqy = (k.buf(n) for n in (
                "rqd", "rk", "rkd", "rqdT", "rkT", "rv", "sgg", "rett", "gB", "rot", "sqy"))
            Bsgt = [k.buf("sgt0"), k.buf("sgt1")]
            BSf = [k.buf("Sf0"), k.buf("Sf1")]
            BSbf = k.buf("Sbf")
            BinT = [k.buf("inT%d" % i) for i in range(4)]
            Bty = [k.buf("ty0"), k.buf("ty1")]
            Bst = [k.buf("st0"), k.buf("st1")]
            k.dma("sp", gB[:], bass.AP(rgain_d.tensor, l * 512, [[0, 128], [1, 512]]), writes=[BgB])

            def load_w(h):
                s = h % 2
                for (c0, w, o) in ((1536 + 64 * h, 64, 0), (1792 + 64 * h, 64, 64), (2048 + 128 * h, 128, 128), (2560 + 128 * h, 128, 256)):
                    k.dma("pool", wr[:, s, :, o:o + w], win_d[l, :, c0:c0 + w].rearrange("(c p) n -> p c n", p=128), writes=[Bw[s]])
            load_w(0)
            for h in range(4):
                s = h % 2
                if h + 1 < 4:
                    load_w(h + 1)
                qd = dec[:, h:h + 1]
                kd = dec[:, 4 + h:5 + h]
                for g4 in range(4):
                    ba = g4 % 2
                    tsl = slice(4 * g4, 4 * g4 + 4)

                    def mm(e):
                        for q4 in range(4):
                            t = 4 * g4 + q4
                            for kc in range(8):
                                ins = e.matmul(ps[:, ba, q4 * 128:(q4 + 1) * 128], lhsT=hT[:, kc, t * 128:(t + 1) * 128], rhs=wr[:, s, kc, 0:128],
                                               start=(kc == 0), stop=(kc == 7))
                        return ins
                    k.op("pe", mm, reads=[Bw[s], B["hT"]], writes=[PB[ba]])
                    pq = ps[:, ba, :].rearrange("p (t a f n) -> p t a f n", t=4, a=2, f=2)
                    t1 = pq[:, :, :, 0, :]
                    t2 = pq[:, :, :, 1, :]
                    cview = cs[:, tsl, 0:32]
                    sview = cs[:, tsl, 32:64]
                    dd = [list(x) for x in cview.ap]
                    cosb = bass.AP(cview.tensor, cview.offset, [dd[0], dd[1], [0, 2], dd[2]])
                    dd = [list(x) for x in sview.ap]
                    sinb = bass.AP(sview.tensor, sview.offset, [dd[0], dd[1], [0, 2], dd[2]])
                    for (ri, ta, tb_) in ((0, t1, cosb), (1, t2, sinb), (2, t1, sinb), (3, t2, cosb)):
                        k.op("dve", lambda e: e.tensor_tensor(out=rot[:, ri], in0=ta, in1=tb_, op=ALU.mult),
                             reads=[PB[ba], B["const"]], writes=[Brot])
                    k.op("dve", lambda e: e.tensor_tensor(out=rot[:, 4], in0=rot[:, 0], in1=rot[:, 1], op=ALU.subtract), reads=[Brot], writes=[Brot])
                    k.op("dve", lambda e: e.tensor_tensor(out=rot[:, 5], in0=rot[:, 2], in1=rot[:, 3], op=ALU.add), reads=[Brot], writes=[Brot])
                    srcq = rot[:, 4:6, :, 0, :].rearrange("p f t n -> p t f n")
                    srck = rot[:, 4:6, :, 1, :].rearrange("p f t n -> p t f n")
                    k.op("act", lambda e: e.activation(out=rqd[:, tsl, :].rearrange("p t (f n) -> p t f n", f=2), in_=srcq, func=AF.Identity, scale=qd),
                         reads=[Brot, B["const"]], writes=[Brqd])
                    k.op("act", lambda e: e.copy(out=rk[:, tsl, :].rearrange("p t (f n) -> p t f n", f=2), in_=srck), reads=[Brot], writes=[Brk])
                    k.op("act", lambda e: e.activation(out=rkd[:, tsl, :].rearrange("p t (f n) -> p t f n", f=2), in_=srck, func=AF.Identity, scale=kd),
                         reads=[Brot], writes=[Brkd])
                    for t2i in range(2):
                        z = (2 * g4 + t2i) % 2
                        bb = 2 + z
                        t0 = 4 * g4 + 2 * t2i

                        def mm2(e):
                            for q2 in range(2):
                                t = t0 + q2
                                for kc in range(8):
                                    ins = e.matmul(ps[:, bb, q2 * 256:(q2 + 1) * 256], lhsT=hT[:, kc, t * 128:(t + 1) * 128], rhs=wr[:, s, kc, 128:384],
                                                   start=(kc == 0), stop=(kc == 7))
                            return ins
                        k.op("pe", mm2, reads=[Bw[s], B["hT"]], writes=[PB[bb]])
                        pv2 = ps[:, bb, :].rearrange("p (q c n) -> p q c n", q=2, c=2)
                        k.op("act", lambda e: e.copy(out=rv[:, t0:t0 + 2, :], in_=pv2[:, :, 0, :]), reads=[PB[bb]], writes=[Brv])
                        k.op("act", lambda e: e.activation(out=sgt[:, z], in_=pv2[:, :, 1, :], func=AF.Silu), reads=[PB[bb]], writes=[Bsgt[z]])
                        k.op("pool", lambda e: e.tensor_tensor(out=sgg[:, t0:t0 + 2, :], in0=sgt[:, z], in1=bcast_mid(gB[:, h * 128:(h + 1) * 128], 2), op=ALU.mult),
                             reads=[Bsgt[z], BgB], writes=[Bsgg])
                gi = 0
                for (src, Bsrc, dstT, BdstT) in ((rqd, Brqd, rqdT, BrqdT), (rk, Brk, rkT, BrkT)):
                    for g4 in range(4):
                        bank = 4 + (gi % 2)
                        gi += 1

                        def tr(e):
                            for q4 in range(4):
                                t = g4 * 4 + q4
                                ins = e.transpose(out=psbf(bank)[0:64, q4 * 128:(q4 + 1) * 128], in_=src[:, t, :], identity=ident[:])
                            return ins
                        k.op("pe", tr, reads=[Bsrc, B["const"]], writes=[PB[bank]])
                        k.op("act", lambda e: e.copy(out=dstT[:, g4 * 512:(g4 + 1) * 512], in_=psbf(bank)[0:64, 0:512]), reads=[PB[bank]], writes=[BdstT])
                for g4 in range(4):
                    bk = 4 + g4

                    def mmkv(e):
                        for q4 in range(4):
                            c = 4 * g4 + q4
                            ins = e.matmul(ps[0:64, bk, q4 * 128:(q4 + 1) * 128], lhsT=rkd[:, c, :], rhs=rv[:, c, :], start=True, stop=True)
                        return ins
                    k.op("pe", mmkv, reads=[Brkd, Brv], writes=[PB[bk]])
                for c in range(NT - 1):
                    bk = 4 + c // 4
                    src = ps[0:64, bk, (c % 4) * 128:(c % 4 + 1) * 128]
                    z = c % 2
                    if c == 0:
                        k.op("dve", lambda e: e.tensor_copy(out=Sf[:, z, :], in_=src), reads=[PB[bk]], writes=[BSf[z]])
                    else:
                        k.op("dve", lambda e: e.scalar_tensor_tensor(out=Sf[:, z, :], in0=Sf[:, 1 - z, :], scalar=CHUNK_DECAY[h], in1=src,
                                                                     op0=ALU.mult, op1=ALU.add), reads=[PB[bk], BSf[1 - z]], writes=[BSf[z]])
                    k.op("act", lambda e: e.copy(out=Sbf[:, c, :], in_=Sf[:, z, :]), reads=[BSf[z]], writes=[BSbf])
                for g4 in range(4):
                    z = g4 % 2
                    yb = 6 + z
                    csl = slice(4 * g4, 4 * g4 + 4)
                    def st_step(q4):
                        c = 4 * g4 + q4
                        cs_ = slice(c * 128, (c + 1) * 128)
                        sbk = 4 + (c % 2)
                        k.op("pe", lambda e: e.matmul(ps[:, sbk, 0:128], lhsT=rkT[:, cs_], rhs=rqdT[:, cs_], start=True, stop=True),
                             reads=[BrkT, BrqdT], writes=[PB[sbk]])
                        k.op("dve", lambda e: e.tensor_tensor(out=inT[:, q4, :], in0=ps[:, sbk, 0:128], in1=mask2[:, h, :], op=ALU.mult),
                             reads=[PB[sbk], B["const"]], writes=[BinT[q4]])

                    def y_step(q4):
                        c = 4 * g4 + q4
                        cs_ = slice(c * 128, (c + 1) * 128)

                        def mmy(e):
                            ins = e.matmul(ps[:, yb, q4 * 128:(q4 + 1) * 128], lhsT=inT[:, q4, :], rhs=rv[:, c, :], start=True, stop=(c == 0))
                            if c > 0:
                                ins = e.matmul(ps[:, yb, q4 * 128:(q4 + 1) * 128], lhsT=rqdT[:, cs_], rhs=Sbf[:, c - 1, :], start=False, stop=True)
                            return ins
                        k.op("pe", mmy, reads=[BinT[q4], Brv, BrqdT, BSbf], writes=[PB[yb]])
                    st_step(0)
                    for q4 in range(4):
                        if q4 + 1 < 4:
                            st_step(q4 + 1)
                        y_step(q4)
                    Y = ps[:, yb, :].rearrange("p (c n) -> p c n", c=4)
                    S1, S2, M, MSQ, VAR, SD, RSTD, NB = (st[:, z, i, :] for i in range(8))
                    k.op("dve", lambda e: e.tensor_reduce(out=S1, in_=Y, axis=AX.X, op=ALU.add), reads=[PB[yb]], writes=[Bst[z]])
                    k.op("act", lambda e: e.activation(out=sqy[:].rearrange("p (c n) -> p c n", c=4), in_=Y, func=AF.Square), reads=[PB[yb]], writes=[Bsqy])
                    k.op("dve", lambda e: e.tensor_reduce(out=S2, in_=sqy[:].rearrange("p (c n) -> p c n", c=4), axis=AX.X, op=ALU.add),
                         reads=[Bsqy, Bst[z]], writes=[Bst[z]])
                    k.op("dve", lambda e: e.tensor_scalar(out=M, in0=S1, scalar1=1.0 / 128.0, scalar2=None, op0=ALU.mult), reads=[Bst[z]], writes=[Bst[z]])
                    k.op("dve", lambda e: e.tensor_tensor(out=MSQ, in0=M, in1=M, op=ALU.mult), reads=[Bst[z]], writes=[Bst[z]])
                    k.op("dve", lambda e: e.scalar_tensor_tensor(out=VAR, in0=S2, scalar=1.0 / 128.0, in1=MSQ, op0=ALU.mult, op1=ALU.subtract),
                         reads=[Bst[z]], writes=[Bst[z]])
                    k.op("act", lambda e: e.activation(out=SD, in_=VAR, func=AF.Sqrt, bias=float(EPS)), reads=[Bst[z]], writes=[Bst[z]])
                    k.op("dve", lambda e: e.reciprocal(out=RSTD, in_=SD), reads=[Bst[z]], writes=[Bst[z]])
                    k.op("dve", lambda e: e.scalar_tensor_tensor(out=NB, in0=M, scalar=-1.0, in1=RSTD, op0=ALU.mult, op1=ALU.mult),
                         reads=[Bst[z]], writes=[Bst[z]])
                    tyv = ty[:, z, :].rearrange("p (c n) -> p c n", c=4)
                    k.op("dve", lambda e: e.tensor_tensor(out=tyv, in0=Y, in1=bcast_last(RSTD, 128), op=ALU.mult), reads=[PB[yb], Bst[z]], writes=[Bty[z]])
                    k.op("dve", lambda e: e.tensor_tensor(out=tyv, in0=tyv, in1=bcast_last(NB, 128), op=ALU.add), reads=[Bty[z], Bst[z]], writes=[Bty[z]])
                    k.op("pool", lambda e: e.tensor_tensor(out=rett[:, csl, :], in0=tyv, in1=sgg[:, csl, :], op=ALU.mult),
                         reads=[Bty[z], Bsgg], writes=[Brett])
                for g4 in range(4):
                    bank = g4 % 2

                    def tr(e):
                        for q4 in range(4):
                            t = g4 * 4 + q4
                            ins = e.transpose(out=psbf(bank)[:, q4 * 128:(q4 + 1) * 128], in_=rett[:, t, :], identity=ident[:])
                        return ins
                    k.op("pe", tr, reads=[Brett, B["const"]], writes=[PB[bank]])
                    k.op("act", lambda e: e.copy(out=retT[:, h, g4 * 512:(g4 + 1) * 512], in_=psbf(bank)[:, 0:512]), reads=[PB[bank]], writes=[BretT])
            k.barrier()

    def ffn(l):
        moe = (l % 2 == 1)
        if moe:
            ctx_g = sb("gates", [128, NT, NEXP], F32)
            gates = ctx_g.__enter__()
            Bgates = k.buf("gates")
            router_logits(l, gates, Bgates)
            experts = [(mw1_d[e], mw3_d[e], mw2_d[e], DFE // 128) for e in range(NEXP)]
        else:
            mod_and_norm(l, "ffn")
            experts = [(w1_d, w3_d, w2_d, DFF // 128)]
        G = 4
        groups = []
        for ei, (a1, a3, a2, nch) in enumerate(experts):
            c0 = 0
            while c0 < nch:
                g = min(G, nch - c0)
                groups.append((ei, a1, a3, a2, c0, g))
                c0 += g
        with (sb("w13", [128, 2, 2, 8, G * 128], BF16) as w13, sb("w2s", [128, 2, G, D], BF16) as w2s,
              sb("gT", [128, 2, G, S], BF16) as gT, sb("sa", [128, 2, 512], BF16) as sa,
              sb("tmpf", [128, 2, D], F32) as tmpf):
            Bw13 = [k.buf("w13_0"), k.buf("w13_1")]
            Bw2 = [k.buf("w2_0"), k.buf("w2_1")]
            BgT = [k.buf("gT0"), k.buf("gT1")]
            Bsa = [k.buf("sa0"), k.buf("sa1")]
            Btf = [k.buf("tmpf0"), k.buf("tmpf1")]

            def load13(gi):
                ei, a1, a3, a2, c0, g = groups[gi]
                s = gi % 2
                k.dma("pool", w13[:, s, 0, :, 0:g * 128], a1[:, c0 * 128:(c0 + g) * 128].rearrange("(c p) n -> p c n", p=128), writes=[Bw13[s]])
                k.dma("pool", w13[:, s, 1, :, 0:g * 128], a3[:, c0 * 128:(c0 + g) * 128].rearrange("(c p) n -> p c n", p=128), writes=[Bw13[s]])

            def load2(gi):
                ei, a1, a3, a2, c0, g = groups[gi]
                s = gi % 2
                k.dma("pool", w2s[:, s, 0:g, :], a2[c0 * 128:(c0 + g) * 128, :].rearrange("(c p) n -> p c n", p=128), writes=[Bw2[s]])

            cnt = [0]

            def up(gi):
                ei, a1, a3, a2, c0, g = groups[gi]
                s = gi % 2
                for j in range(g):
                    for tb in range(4):
                        i2 = cnt[0] % 2
                        cnt[0] += 1
                        ba, bb = 2 * i2, 2 * i2 + 1

                        def mm(e):
                            for w, bk in ((0, ba), (1, bb)):
                                for kc in range(8):
                                    ins = e.matmul(ps[:, bk, :], lhsT=w13[:, s, w, kc, j * 128:(j + 1) * 128], rhs=hT[:, kc, tb * 512:(tb + 1) * 512],
                                                   start=(kc == 0), stop=(kc == 7))
                            return ins
                        k.op("pe", mm, reads=[Bw13[s], B["hT"]], writes=[PB[ba], PB[bb]])
                        k.op("act", lambda e: e.activation(out=sa[:, i2, :], in_=ps[:, ba, :], func=AF.Silu), reads=[PB[ba]], writes=[Bsa[i2]])
                        k.op("dve", lambda e: e.tensor_tensor(out=gT[:, s, j, tb * 512:(tb + 1) * 512], in0=ps[:, bb, :], in1=sa[:, i2, :], op=ALU.mult),
                             reads=[PB[bb], Bsa[i2]], writes=[BgT[s]])

            def down(gi):
                ei, a1, a3, a2, c0, g = groups[gi]
                s = gi % 2
                for t in range(NT):
                    b0 = 4 + 2 * (t % 2)

                    def mm(e):
                        for hf in range(2):
                            for j in range(g):
                                ins = e.matmul(ps[:, b0 + hf, :], lhsT=gT[:, s, j, t * 128:(t + 1) * 128], rhs=w2s[:, s, j, hf * 512:(hf + 1) * 512],
                                               start=(j == 0), stop=(j == g - 1))
                        return ins
                    k.op("pe", mm, reads=[BgT[s], Bw2[s]], writes=[PB[b0], PB[b0 + 1]])
                    sc = (gates[:, t, ei:ei + 1], Bgates) if moe else None
                    residual_update(t, None, (b0, b0 + 1), tmpf[:, t % 2, :], Btf[t % 2], scalar=sc)
                    if gi == len(groups) - 1 and (l, "ffn") == tuple(sublayers[-1]):
                        k.dma("sp", out_d[t * 128:(t + 1) * 128, :], x[:, t, :], reads=[XT[t]], writes=[out_buf])
                        stored.add(t)

            load13(0)
            load2(0)
            for gi in range(len(groups)):
                if gi + 1 < len(groups):
                    load13(gi + 1)
                up(gi)
                if gi > 0:
                    down(gi - 1)
                if gi + 1 < len(groups):
                    load2(gi + 1)
            down(len(groups) - 1)
            k.barrier()
        if moe:
            ctx_g.__exit__(None, None, None)

    def router_logits(l, gates, Bgates):
        with (sb("rt", [128, 8, NEXP], F32) as rt, sb("rhl", [128, 2, 8, NEXP], BF16) as rhl,
              sb("rtmp", [128, 8, NEXP], F32) as rtmp, sb("lo", [128, 2, D], BF16) as lo,
              sb("loT", [128, 2, 8, 128], BF16) as loT, sb("lg", [128, NT, NEXP], F32) as lg,
              sb("tk", [128, 8, NT, NEXP], F32) as tk, sb("tm", [128, 8, NT], F32) as tm):
            Brt, Blg, Btk = k.buf("rt"), k.buf("lg"), k.buf("tk")
            Blo = [k.buf("lo0"), k.buf("lo1")]
            BloT = [k.buf("loT0"), k.buf("loT1")]
            k.dma("sp", rt[:], router_d.rearrange("(c p) e -> p c e", p=128), writes=[Brt])
            k.op("dve", lambda e: e.tensor_copy(out=rhl[:, 0], in_=rt[:]), reads=[Brt], writes=[Brt])
            k.op("dve", lambda e: e.tensor_tensor(out=rtmp[:], in0=rt[:], in1=rhl[:, 0], op=ALU.subtract), reads=[Brt], writes=[Brt])
            k.op("dve", lambda e: e.tensor_copy(out=rhl[:, 1], in_=rtmp[:]), reads=[Brt], writes=[Brt])

            def per_tile(t, tmp, s2, Btmp, hbf, Bhbf):
                k.op("dve", lambda e: e.tensor_tensor(out=lo[:, s2, :], in0=tmp[:, s2, :], in1=hbf[:, s2, :], op=ALU.subtract),
                     reads=[Btmp, Bhbf], writes=[Blo[s2]])
                bank = 4 + s2

                def tr(e):
                    for kc in range(8):
                        ins = e.transpose(out=psbf(bank)[:, kc * 128:(kc + 1) * 128], in_=lo[:, s2, kc * 128:(kc + 1) * 128], identity=ident[:])
                    return ins
                k.op("pe", tr, reads=[Blo[s2], B["const"]], writes=[PB[bank]])
                k.op("act", lambda e: e.copy(out=loT[:, s2], in_=psbf(bank).rearrange("p (c n) -> p c n", c=8)), reads=[PB[bank]], writes=[BloT[s2]])
                lb = 6 + s2

                def mm(e):
                    n = 0
                    for (lh, rr) in ((0, 0), (0, 1), (1, 0)):
                        for kc in range(8):
                            lt = hT[:, kc, t * 128:(t + 1) * 128] if lh == 0 else loT[:, s2, kc, :]
                            ins = e.matmul(ps[:, lb, 0:NEXP], lhsT=lt, rhs=rhl[:, rr, kc, :], start=(n == 0), stop=(n == 23))
                            n += 1
                    return ins
                k.op("pe", mm, reads=[B["hT"], BloT[s2], Brt], writes=[PB[lb]])
                k.op("dve", lambda e: e.tensor_copy(out=lg[:, t, :], in_=ps[:, lb, 0:NEXP]), reads=[PB[lb]], writes=[Blg])
            mod_and_norm(l, "ffn", want_f32=per_tile)
            m1, m2, dd, w1, w2 = tm[:, 0], tm[:, 1], tm[:, 2], tm[:, 3], tm[:, 4]
            eq1, l2, eq2, g1, g2 = tk[:, 0], tk[:, 1], tk[:, 2], tk[:, 3], tk[:, 4]

            def dv(fn, reads=(Blg,), writes=(Btk,)):
                k.op("dve", fn, reads=list(reads) + [Btk], writes=list(writes))
            dv(lambda e: e.tensor_reduce(out=m1, in_=lg[:], axis=AX.X, op=ALU.max))
            dv(lambda e: e.tensor_tensor(out=eq1, in0=lg[:], in1=bcast_last(m1, NEXP), op=ALU.is_equal))
            dv(lambda e: e.scalar_tensor_tensor(out=l2, in0=eq1, scalar=-1e30, in1=lg[:], op0=ALU.mult, op1=ALU.add))
            dv(lambda e: e.tensor_reduce(out=m2, in_=l2, axis=AX.X, op=ALU.max))
            dv(lambda e: e.tensor_tensor(out=eq2, in0=l2, in1=bcast_last(m2, NEXP), op=ALU.is_equal))
            dv(lambda e: e.tensor_tensor(out=dd, in0=m1, in1=m2, op=ALU.subtract))
            k.op("act", lambda e: e.activation(out=w1, in_=dd, func=AF.Sigmoid), reads=[Btk], writes=[Btk])
            dv(lambda e: e.tensor_scalar(out=w2, in0=w1, scalar1=-1.0, scalar2=1.0, op0=ALU.mult, op1=ALU.add))
            dv(lambda e: e.tensor_tensor(out=g1, in0=eq1, in1=bcast_last(w1, NEXP), op=ALU.mult))
            dv(lambda e: e.tensor_tensor(out=g2, in0=eq2, in1=bcast_last(w2, NEXP), op=ALU.mult))
            k.op("dve", lambda e: e.tensor_tensor(out=gates[:], in0=g1, in1=g2, op=ALU.add), reads=[Btk], writes=[Bgates])
            k.barrier()

    stored = set()
    for (l, sub) in sublayers:
        if stop == "setup":
            break
        if sub == "mix":
            mod_and_norm(l, "mix")
            mixer(l)
        else:
            ffn(l)

    if len(stored) < NT:
        for t4 in range(4):
            k.dma("sp", out_d[512 * t4:512 * (t4 + 1), :].rearrange("(t p) d -> p t d", p=128), x[:, 4 * t4:4 * t4 + 4, :],
                  reads=XT[4 * t4:4 * t4 + 4], writes=[out_buf])
    E = k.E["sp"]
    E.h.wait_ge(out_buf.dsem, 16 * out_buf.dcount)
    nc.used_inputs = sorted(used)
    return nc


_CONSTS = None


def make_in_maps(inputs):
    global _CONSTS
    if _CONSTS is None:
        _CONSTS = host_consts()
    f = lambda a: np.ascontiguousarray(np.asarray(a, dtype=np.float32))
    shared = {
        "rel_bias_table": f(inputs["rel_bias_table"]),
        "norm_mix": f(inputs["norm_mix"]), "norm_ffn": f(inputs["norm_ffn"]),
        "w_mod": f(inputs["w_mod"]), "b_mod": f(inputs["b_mod"]), "w_in": f(inputs["w_in"]),
        "q_gain2": f(np.tile(np.asarray(inputs["q_gain"]), (1, 2)).T),
        "k_gain2": f(np.tile(np.asarray(inputs["k_gain"]), (1, 2)).T),
        "ret_gain": f(inputs["ret_gain"]), "w_out": f(inputs["w_out"]),
        "ffn_w1": f(np.asarray(inputs["ffn_w1"])[0]), "ffn_w3": f(np.asarray(inputs["ffn_w3"])[0]), "ffn_w2": f(np.asarray(inputs["ffn_w2"])[0]),
        "moe_router": f(np.asarray(inputs["moe_router"])[0]),
        "moe_w1": f(np.asarray(inputs["moe_w1"])[0]), "moe_w3": f(np.asarray(inputs["moe_w3"])[0]), "moe_w2": f(np.asarray(inputs["moe_w2"])[0]),
    }
    shared.update(_CONSTS)
    xs = np.asarray(inputs["x"], dtype=np.float32)
    cc = np.asarray(inputs["c"], dtype=np.float32)
    maps = []
    for b in range(8):
        m = dict(shared)
        m["x"] = np.ascontiguousarray(xs[b])
        m["cT"] = np.ascontiguousarray(cc[b].reshape(8, 128).T)
        maps.append(m)
    return maps


def kernel(**inputs):
    nc = build()
    maps = [{n: m[n] for n in nc.used_inputs} for m in make_in_maps(inputs)]
    res = run_bass_kernel_spmd(nc, maps, core_ids=list(range(8)))
    return np.stack([np.asarray(r["out"], dtype=np.float32) for r in res.results], axis=0)
```
